# Optimizing a Trainium2 kernel written in Bass

```python
import jax
import jax.numpy as jnp
from jax import lax
import numpy as np


D_MODEL = 2048
BATCH = 16
SEQ = 2048
DEPTH = 2

EPS = 1e-6
M_HEADS = 4
M_HEAD_DIM = D_MODEL // 16
M_WIDTH = M_HEADS * M_HEAD_DIM
M_CONV = 4
M_CHUNK = 64
M_FGATE_BIAS = 3.0
S_HEADS = 16
S_HEAD_DIM = D_MODEL // 32
S_WIDTH = S_HEADS * S_HEAD_DIM
S_GROUPS = 2
S_STATE = 128
S_CONV = 4
S_CHUNK = 128
S_XBC = S_WIDTH + 2 * S_GROUPS * S_STATE
A_HEADS = 4
A_HEAD_DIM = D_MODEL // 16
A_WIDTH = A_HEADS * A_HEAD_DIM
A_BLOCK = 256
A_TOPK = 3
A_QCHUNK = 32
D_MIX = M_WIDTH + S_WIDTH + A_WIDTH
PROJ_SIZES = (M_WIDTH, M_WIDTH, M_WIDTH, M_WIDTH, M_HEADS, M_HEADS,
              S_WIDTH, S_XBC, S_HEADS,
              A_WIDTH, A_WIDTH, A_WIDTH)
D_PROJ = sum(PROJ_SIZES)
D_FF = D_MODEL * 11 // 4
N_EXPERTS = 8
TOP_K = 2
D_FF_EXPERT = D_MODEL * 7 // 2
N_DENSE = (DEPTH + 1) // 2
N_MOE = DEPTH // 2
PLE_DIM = 256

kernel_name = "hybrid_mlstm_ssd_moba_moe_block"

F32 = jnp.float32


def rmsnorm(x, g):
    xf = x.astype(F32)
    y = xf * lax.rsqrt(jnp.mean(xf * xf, axis=-1, keepdims=True) + EPS)
    return (y * g.astype(F32)).astype(x.dtype)


def causal_dwconv(x, w, b):
    k, c = w.shape
    y = lax.conv_general_dilated(x, w[:, None, :].astype(x.dtype), window_strides=(1,),
                                 padding=((k - 1, 0),), dimension_numbers=('NWC', 'WIO', 'NWC'),
                                 feature_group_count=c)
    return y + b.astype(x.dtype)


def mlstm_mixer(q, k, v, o, i_pre, f_pre, conv_w, conv_b, gate_b, norm_g):
    bsz, seq, _ = q.shape
    H, Dh, L = M_HEADS, M_HEAD_DIM, M_CHUNK
    nc = seq // L
    qk = jax.nn.silu(causal_dwconv(jnp.concatenate([q, k], axis=-1), conv_w, conv_b))
    q, k = jnp.split(qk.astype(F32), 2, axis=-1)
    k = k * (Dh ** -0.5)

    def to_chunks(t):
        return t.reshape(bsz, nc, L, H, Dh).transpose(1, 0, 3, 2, 4)

    def gate_chunks(t):
        return t.reshape(bsz, nc, L, H).transpose(1, 0, 3, 2)

    li = gate_chunks(i_pre.astype(F32) + gate_b[:H].astype(F32))
    lf = gate_chunks(jax.nn.log_sigmoid(f_pre.astype(F32) + gate_b[H:].astype(F32)))
    causal = jnp.tril(jnp.ones((L, L), dtype=bool))

    def step(carry, inp):
        C, n, m = carry
        qc, kc, vc, lic, lfc = inp
        b = jnp.cumsum(lfc, axis=-1)
        logw = jnp.where(causal, b[..., :, None] - b[..., None, :] + lic[..., None, :], -jnp.inf)
        m_inter = b + m[..., None]
        m_j = jnp.maximum(m_inter, logw.max(axis=-1))
        w = jnp.exp(logw - m_j[..., None])
        s_inter = jnp.exp(m_inter - m_j)
        sqk = jnp.einsum('bhjd,bhsd->bhjs', qc, kc) * w
        num = s_inter[..., None] * jnp.einsum('bhvk,bhjk->bhjv', C, qc) + jnp.einsum('bhjs,bhsv->bhjv', sqk, vc)
        den = s_inter * jnp.einsum('bhk,bhjk->bhj', n, qc) + sqk.sum(axis=-1)
        h = num / jnp.maximum(jnp.abs(den), jnp.exp(-m_j))[..., None]
        total = b[..., -1]
        lw_end = total[..., None] - b + lic
        m_new = jnp.maximum(total + m, lw_end.max(axis=-1))
        a = jnp.exp(lw_end - m_new[..., None])
        decay = jnp.exp(total + m - m_new)
        C = decay[..., None, None] * C + jnp.einsum('bhs,bhsv,bhsk->bhvk', a, vc, kc)
        n = decay[..., None] * n + jnp.einsum('bhs,bhsk->bhk', a, kc)
        return (C, n, m_new), h

    init = (jnp.zeros((bsz, H, Dh, Dh), F32), jnp.zeros((bsz, H, Dh), F32), jnp.zeros((bsz, H), F32))
    _, h = lax.scan(step, init, (to_chunks(q), to_chunks(k), to_chunks(v.astype(F32)), li, lf))
    h = h.transpose(1, 0, 3, 2, 4).reshape(bsz, seq, H, Dh)
    h = h * lax.rsqrt(jnp.mean(h * h, axis=-1, keepdims=True) + EPS)
    return h.reshape(bsz, seq, M_WIDTH) * norm_g.astype(F32) * jax.nn.sigmoid(o.astype(F32))


def mamba2_mixer(z, xbc, dt_raw, conv_w, conv_b, dt_bias, a_log, d_skip, norm_g):
    bsz, seq, _ = z.shape
    L, G, E, P, N = S_CHUNK, S_GROUPS, S_HEADS // S_GROUPS, S_HEAD_DIM, S_STATE
    nc = seq // L
    xbc = jax.nn.silu(causal_dwconv(xbc, conv_w, conv_b)).astype(F32)
    xs, bm, cm = jnp.split(xbc, [S_WIDTH, S_WIDTH + G * N], axis=-1)
    X = xs.reshape(bsz, nc, L, G, E, P)
    Bc = bm.reshape(bsz, nc, L, G, N)
    Cc = cm.reshape(bsz, nc, L, G, N)
    dt = jax.nn.softplus(dt_raw.astype(F32) + dt_bias.astype(F32)).reshape(bsz, nc, L, G, E)
    a = dt * (-jnp.exp(a_log.astype(F32))).reshape(G, E)
    acum = jnp.cumsum(a, axis=2)
    Xdt = X * dt[..., None]
    causal = jnp.tril(jnp.ones((L, L), dtype=bool))[:, :, None, None]
    seg = acum[:, :, :, None] - acum[:, :, None, :]
    Lmat = jnp.exp(jnp.where(causal, seg, -jnp.inf))
    CB = jnp.einsum('bclgn,bcsgn->bclsg', Cc, Bc)
    y_diag = jnp.einsum('bclsge,bcsgep->bclgep', CB[..., None] * Lmat, Xdt)
    decay_to_end = jnp.exp(acum[:, :, -1:] - acum)
    states = jnp.einsum('bclgn,bclgep->bcgepn', Bc, Xdt * decay_to_end[..., None])
    chunk_decay = jnp.exp(acum[:, :, -1])

    def step(h, inp):
        st, dec = inp
        return h * dec[..., None, None] + st, h

    _, h_in = lax.scan(step, jnp.zeros((bsz, G, E, P, N), F32),
                       (jnp.moveaxis(states, 1, 0), jnp.moveaxis(chunk_decay, 1, 0)))
    h_in = jnp.moveaxis(h_in, 0, 1)
    y_off = jnp.einsum('bclgn,bcgepn->bclgep', Cc, h_in) * jnp.exp(acum)[..., None]
    y = y_diag + y_off + X * d_skip.astype(F32).reshape(G, E, 1)
    gw = S_WIDTH // G
    y = y.reshape(bsz, seq, G, gw) * jax.nn.silu(z.astype(F32)).reshape(bsz, seq, G, gw)
    y = y * lax.rsqrt(jnp.mean(y * y, axis=-1, keepdims=True) + EPS)
    return y.reshape(bsz, seq, S_WIDTH) * norm_g.astype(F32)


def moba_mixer(q, k, v):
    bsz, seq, _ = q.shape
    H, Dh, BS, QC = A_HEADS, A_HEAD_DIM, A_BLOCK, A_QCHUNK
    nb = -(-seq // BS)
    pad = nb * BS - seq
    nqc = seq // QC
    k_sel = max(1, min(A_TOPK, nb - 1))

    def heads(t):
        return t.astype(F32).reshape(bsz, seq, H, Dh).transpose(0, 2, 1, 3)

    q = heads(q) * (Dh ** -0.5)
    kb = jnp.pad(heads(k), ((0, 0), (0, 0), (0, pad), (0, 0))).reshape(bsz, H, nb, BS, Dh)
    vb = jnp.pad(heads(v), ((0, 0), (0, 0), (0, pad), (0, 0))).reshape(bsz, H, nb, BS, Dh)
    k_mean = kb.mean(axis=3)
    q_blk = jnp.arange(seq) // BS
    past = jnp.arange(nb)[None, :] < q_blk[:, None]
    blk_score = jnp.where(past, jnp.einsum('bhsd,bhnd->bhsn', q, k_mean), -jnp.inf)
    _, sel = lax.top_k(blk_score, k_sel)
    valid = jnp.arange(k_sel)[None, :] < q_blk[:, None]
    gather = jax.vmap(jax.vmap(lambda blocks, idx: blocks[idx]))

    def chunk(args):
        qc, selc, validc, c = args
        t0 = c * QC
        bt = t0 // BS
        k_g = gather(kb, selc)
        v_g = gather(vb, selc)
        s_sel = jnp.einsum('bhqd,bhqrkd->bhqrk', qc, k_g)
        s_sel = jnp.where(validc[None, None, :, :, None], s_sel, -jnp.inf).reshape(bsz, H, QC, k_sel * BS)
        k_own = lax.dynamic_index_in_dim(kb, bt, axis=2, keepdims=False)
        v_own = lax.dynamic_index_in_dim(vb, bt, axis=2, keepdims=False)
        own_mask = (bt * BS + jnp.arange(BS))[None, :] <= (t0 + jnp.arange(QC))[:, None]
        s_own = jnp.where(own_mask, jnp.einsum('bhqd,bhkd->bhqk', qc, k_own), -jnp.inf)
        probs = jax.nn.softmax(jnp.concatenate([s_sel, s_own], axis=-1), axis=-1)
        p_sel = probs[..., :k_sel * BS].reshape(bsz, H, QC, k_sel, BS)
        p_own = probs[..., k_sel * BS:]
        return jnp.einsum('bhqrk,bhqrkd->bhqd', p_sel, v_g) + jnp.einsum('bhqk,bhkd->bhqd', p_own, v_own)

    xs = (jnp.moveaxis(q.reshape(bsz, H, nqc, QC, Dh), 2, 0),
          jnp.moveaxis(sel.reshape(bsz, H, nqc, QC, k_sel), 2, 0),
          valid.reshape(nqc, QC, k_sel),
          jnp.arange(nqc))
    out = lax.map(chunk, xs)
    out = jnp.moveaxis(out, 0, 2).reshape(bsz, H, seq, Dh).transpose(0, 2, 1, 3)
    return out.reshape(bsz, seq, A_WIDTH)


def swiglu(x, wg, wu, wd):
    return (jax.nn.silu(x @ wg) * (x @ wu)) @ wd


def moe_swiglu(x, w_router, w_gate, w_up, w_down):
    bsz, seq, d = x.shape
    xt = x.reshape(bsz * seq, d)
    logits = (xt @ w_router).astype(F32)
    top_logit, top_idx = lax.top_k(logits, TOP_K)
    top_w = jax.nn.softmax(top_logit, axis=-1)
    combine = jnp.einsum('tk,tke->te', top_w, jax.nn.one_hot(top_idx, N_EXPERTS, dtype=F32)).astype(x.dtype)
    out = jnp.zeros_like(xt)
    for e in range(N_EXPERTS):
        out = out + combine[:, e:e + 1] * swiglu(xt, w_gate[e], w_up[e], w_down[e])
    return out.reshape(bsz, seq, d)


def setup_inputs(seed: int = 0) -> dict:
    key = jax.random.key(seed)
    ks = iter(jax.random.split(key, 40))

    def nrm(shape, scale):
        return jax.random.normal(next(ks), shape, F32) * scale

    def gain(shape):
        return 1.0 + 0.02 * jax.random.normal(next(ks), shape, F32)

    dt0 = jnp.exp(jax.random.uniform(next(ks), (DEPTH, S_HEADS), F32) * (np.log(0.1) - np.log(0.001)) + np.log(0.001))
    return {
        'x': nrm((BATCH, SEQ, D_MODEL), 1.0),
        'p': nrm((DEPTH, BATCH, SEQ, PLE_DIM), 1.0),
        'ln_mix': gain((DEPTH, D_MODEL)),
        'w_in': nrm((DEPTH, D_MODEL, D_PROJ), D_MODEL ** -0.5),
        'w_out': nrm((DEPTH, D_MIX, D_MODEL), D_MIX ** -0.5),
        'm_conv_w': nrm((DEPTH, M_CONV, 2 * M_WIDTH), M_CONV ** -0.5),
        'm_conv_b': nrm((DEPTH, 2 * M_WIDTH), 0.02),
        'm_gate_b': jnp.concatenate([nrm((DEPTH, M_HEADS), 0.1),
                                     M_FGATE_BIAS + nrm((DEPTH, M_HEADS), 0.5)], axis=-1),
        'm_norm_g': gain((DEPTH, M_WIDTH)),
        's_conv_w': nrm((DEPTH, S_CONV, S_XBC), S_CONV ** -0.5),
        's_conv_b': nrm((DEPTH, S_XBC), 0.02),
        's_dt_bias': dt0 + jnp.log(-jnp.expm1(-dt0)),
        's_a_log': jnp.log(jax.random.uniform(next(ks), (DEPTH, S_HEADS), F32, 1.0, 16.0)),
        's_d': 1.0 + nrm((DEPTH, S_HEADS), 0.1),
        's_norm_g': gain((DEPTH, S_WIDTH)),
        'ln_ffn': gain((DEPTH, D_MODEL)),
        'ffn_w_gate': nrm((N_DENSE, D_MODEL, D_FF), D_MODEL ** -0.5),
        'ffn_w_up': nrm((N_DENSE, D_MODEL, D_FF), D_MODEL ** -0.5),
        'ffn_w_down': nrm((N_DENSE, D_FF, D_MODEL), D_FF ** -0.5),
        'moe_router': nrm((N_MOE, D_MODEL, N_EXPERTS), D_MODEL ** -0.5),
        'moe_w_gate': nrm((N_MOE, N_EXPERTS, D_MODEL, D_FF_EXPERT), D_MODEL ** -0.5),
        'moe_w_up': nrm((N_MOE, N_EXPERTS, D_MODEL, D_FF_EXPERT), D_MODEL ** -0.5),
        'moe_w_down': nrm((N_MOE, N_EXPERTS, D_FF_EXPERT, D_MODEL), D_FF_EXPERT ** -0.5),
        'ln_ple': gain((DEPTH, D_MODEL)),
        'ple_proj': nrm((DEPTH, PLE_DIM, D_MODEL), PLE_DIM ** -0.5),
        'ple_gate': nrm((DEPTH, D_MODEL, D_MODEL), D_MODEL ** -0.5),
        'ln_final': gain((D_MODEL,)),
    }


def reference(x, p, ln_mix, w_in, w_out, m_conv_w, m_conv_b, m_gate_b, m_norm_g,
              s_conv_w, s_conv_b, s_dt_bias, s_a_log, s_d, s_norm_g,
              ln_ffn, ffn_w_gate, ffn_w_up, ffn_w_down,
              moe_router, moe_w_gate, moe_w_up, moe_w_down,
              ln_ple, ple_proj, ple_gate, ln_final):
    split_idx = [int(s) for s in np.cumsum(PROJ_SIZES)[:-1]]
    for i in range(DEPTH):
        u = rmsnorm(x, ln_mix[i]) @ w_in[i]
        (mq, mk, mv, mo, mi, mf, sz, sxbc, sdt, aq, ak, av) = jnp.split(u, split_idx, axis=-1)
        y_m = mlstm_mixer(mq, mk, mv, mo, mi, mf, m_conv_w[i], m_conv_b[i], m_gate_b[i], m_norm_g[i])
        y_s = mamba2_mixer(sz, sxbc, sdt, s_conv_w[i], s_conv_b[i], s_dt_bias[i], s_a_log[i], s_d[i], s_norm_g[i])
        y_a = moba_mixer(aq, ak, av)
        mixed = jnp.concatenate([y_m.astype(x.dtype), y_s.astype(x.dtype), y_a.astype(x.dtype)], axis=-1)
        x = x + mixed @ w_out[i]
        hn = rmsnorm(x, ln_ffn[i])
        if i % 2 == 0:
            j = i // 2
            x = x + swiglu(hn, ffn_w_gate[j], ffn_w_up[j], ffn_w_down[j])
        else:
            j = i // 2
            x = x + moe_swiglu(hn, moe_router[j], moe_w_gate[j], moe_w_up[j], moe_w_down[j])
        gate = jax.nn.sigmoid(rmsnorm(x, ln_ple[i]) @ ple_gate[i])
        x = x + (p[i] @ ple_proj[i]) * gate
    return rmsnorm(x, ln_final)
```

```python
from contextlib import ExitStack
import numpy as np
import ml_dtypes
import concourse.bass as bass
import concourse.mybir as mybir
from concourse.bass_utils import run_bass_kernel_spmd
from concourse.alu_op_type import AluOpType as ALU

F32 = mybir.dt.float32
BF16 = mybir.dt.bfloat16
AF = mybir.ActivationFunctionType
AX = mybir.AxisListType

N_DMA_SEMS = 12
D = 2048
KC = 16
DPROJ = 6168
EPS = 1e-6
NEG = -30000.0


class Buf:
    __slots__ = ("t", "name", "writers", "readers", "gen_war")

    def __init__(self, t, name=""):
        self.t = t
        self.name = name
        self.writers = []
        self.readers = []
        self.gen_war = []

    def __getitem__(self, k):
        return self.t[k]


class Eng:
    def __init__(self, name):
        self.name = name
        self.sem = None
        self.count = 0
        self.ops = []
        self.waited = {}
        self.dma_sems = []
        self.dma_vals = []
        self.dma_i = 0


class Prog:
    def __init__(self, nc):
        self.nc = nc
        self.es = ExitStack()
        self.root_es = self.es
        self.engs = {n: Eng(n) for n in ("pe", "act", "dve", "pool", "sp")}
        for n, e in self.engs.items():
            e.sem = self.es.enter_context(nc.semaphore("c_" + n))
        for n in ("sp", "pool", "act"):
            e = self.engs[n]
            for i in range(N_DMA_SEMS):
                e.dma_sems.append(self.es.enter_context(nc.semaphore(f"d_{n}{i}")))
                e.dma_vals.append(0)
        self.all_dma_events = []
        self.nbuf = 0

    def sbuf(self, shape, dtype, name=None):
        self.nbuf += 1
        name = (name or "sb") + f"_{self.nbuf}"
        t = self.es.enter_context(self.nc.sbuf_tensor(name, list(shape), dtype))
        return Buf(t, name)

    def psum(self, shape, dtype, name=None):
        self.nbuf += 1
        name = (name or "ps") + f"_{self.nbuf}"
        t = self.es.enter_context(self.nc.psum_tensor(name, list(shape), dtype))
        return Buf(t, name)

    @staticmethod
    def _continuing(engname, b):
        return (engname.startswith("dma_") and b.writers and not b.readers
                and all(w[0].startswith("dma_") for w in b.writers))

    def _deps(self, engname, reads, writes):
        deps = {}
        is_dma = engname.startswith("dma_")

        def add(ev, raw):
            en, sem, val = ev
            if en == engname and not is_dma:
                if not raw or engname == "pe":
                    return
            k = id(sem)
            if k not in deps or deps[k][1] < val:
                deps[k] = (sem, val)
        for b in reads:
            for w in b.writers:
                add(w, True)
        for b in writes:
            if self._continuing(engname, b):
                for w in b.gen_war:
                    add(w, False)
            else:
                for w in b.writers:
                    add(w, False)
                for r in b.readers:
                    add(r, False)
        return deps

    def _emit_waits(self, eng, deps):
        waits = []
        for k, (sem, val) in deps.items():
            if eng.waited.get(k, 0) >= val:
                continue
            eng.waited[k] = val
            waits.append((sem, val))
        return waits

    def _record(self, engname, ev, reads, writes):
        for b in writes:
            if self._continuing(engname, b):
                b.writers.append(ev)
            else:
                b.gen_war = b.writers + b.readers
                b.writers = [ev]
                b.readers = []
        for b in reads:
            if not engname.startswith("dma_"):
                b.readers = [r for r in b.readers if r[0] != engname]
            b.readers.append(ev)

    def op(self, engname, fn, reads=(), writes=()):
        eng = self.engs[engname]
        waits = self._emit_waits(eng, self._deps(engname, reads, writes))
        eng.count += 1
        val = eng.count
        sem = eng.sem

        def run(e, waits=waits, fn=fn, sem=sem):
            for s, v in waits:
                e.wait_ge(s, v)
            fn(e).then_inc(sem, 1)
        eng.ops.append(run)
        ev = (engname, sem, val)
        self._record(engname, ev, reads, writes)
        return ev

    def dma(self, qname, out, in_, reads=(), writes=(), **kw):
        eng = self.engs[qname]
        deps = self._deps("dma_" + qname, reads, writes)
        i = eng.dma_i % N_DMA_SEMS
        eng.dma_i += 1
        sem = eng.dma_sems[i]
        prev = eng.dma_vals[i]
        if prev:
            k = id(sem)
            if k not in deps or deps[k][1] < prev:
                deps[k] = (sem, prev)
        waits = self._emit_waits(eng, deps)
        val = prev + 16
        eng.dma_vals[i] = val

        def run(e, waits=waits, sem=sem, out=out, in_=in_, kw=kw):
            for s, v in waits:
                e.wait_ge(s, v)
            e.dma_start(out=out, in_=in_, **kw).then_inc(sem, 16)
        eng.ops.append(run)
        ev = ("dma_" + qname, sem, val)
        self._record("dma_" + qname, ev, reads, writes)
        self.all_dma_events.append((sem, val))
        return ev

    def _all_events(self):
        evs = {}
        for sem, val in self.all_dma_events:
            k = id(sem)
            if k not in evs or evs[k][1] < val:
                evs[k] = (sem, val)
        self.all_dma_events = list(evs.values())
        for n, e in self.engs.items():
            if e.count:
                evs[id(e.sem)] = (e.sem, e.count)
        return evs

    def barrier(self):
        evs = self._all_events()
        for n, e in self.engs.items():
            waits = self._emit_waits(e, {k: v for k, v in evs.items() if k != id(e.sem)})
            if waits:
                def run(en, waits=waits):
                    for s, v in waits:
                        en.wait_ge(s, v)
                e.ops.append(run)

    def flush(self):
        nc = self.nc
        engs = self.engs
        with nc.Block() as block:
            @block.tensor
            def _(e):
                for f in engs["pe"].ops:
                    f(e)

            @block.scalar
            def _(e):
                for f in engs["act"].ops:
                    f(e)

            @block.vector
            def _(e):
                for f in engs["dve"].ops:
                    f(e)

            @block.gpsimd
            def _(e):
                for f in engs["pool"].ops:
                    f(e)

            @block.sync
            def _(e):
                for f in engs["sp"].ops:
                    f(e)
        for e in engs.values():
            e.ops = []

    def scope(self):
        prog = self

        class _S:
            def __enter__(s):
                s.saved = prog.es
                prog.es = ExitStack()
                return prog

            def __exit__(s, *a):
                if a[0] is None:
                    prog.barrier()
                    prog.flush()
                prog.es.close()
                prog.es = s.saved
                return False
        return _S()

    def finish(self):
        self.barrier()
        self.flush()
        self.es.close()


def mm(p, out, lhsT, rhs, start, stop, reads, writes):
    p.op("pe", lambda e: e.matmul(out, lhsT, rhs, start=start, stop=stop), reads, writes)


def tr(p, out, in_, ident, reads, writes):
    p.op("pe", lambda e: e.transpose(out, in_, ident), reads, writes)


def act(p, out, in_, func, reads, writes, **kw):
    p.op("act", lambda e: e.activation(out=out, in_=in_, func=func, **kw), reads, writes)


def tt(p, out, in0, in1, op, reads, writes, eng="dve"):
    p.op(eng, lambda e: e.tensor_tensor(out=out, in0=in0, in1=in1, op=op), reads, writes)


def ts(p, out, in0, s1, s2, op0, op1, reads, writes, eng="dve"):
    if op1 is None:
        p.op(eng, lambda e: e.tensor_scalar(out=out, in0=in0, scalar1=s1, scalar2=None, op0=op0), reads, writes)
    else:
        p.op(eng, lambda e: e.tensor_scalar(out=out, in0=in0, scalar1=s1, scalar2=s2, op0=op0, op1=op1), reads, writes)


def stt(p, out, in0, scalar, in1, op0, op1, reads, writes):
    p.op("dve", lambda e: e.scalar_tensor_tensor(out=out, in0=in0, scalar=scalar, in1=in1, op0=op0, op1=op1),
         reads, writes)


def cp(p, out, in_, reads, writes, eng="dve"):
    if eng == "act":
        p.op(eng, lambda e: e.activation(out=out, in_=in_, func=AF.Copy), reads, writes)
    else:
        p.op(eng, lambda e: e.tensor_copy(out=out, in_=in_), reads, writes)


def recip(p, out, in_, reads, writes):
    p.op("dve", lambda e: e.reciprocal(out=out, in_=in_), reads, writes)


def mset(p, ap, val, writes, eng="dve"):
    p.op(eng, lambda e: e.memset(ap, val), (), writes)


class Ctx:
    pass


def norm_tile(p, c, xt, gcols, xn_out_fn, sq, ps, tmp, rstd, xn_bufs):
    act(p, sq[:], xt[:], AF.Square, [xt], [sq])
    for kc in range(KC):
        mm(p, ps[:], c.ones_bf, sq[:, kc, :], kc == 0, kc == KC - 1, [sq, c.cb], [ps])
    act(p, tmp[:], ps[:], AF.Sqrt, [ps, c.cvb], [tmp], scale=1.0 / D, bias=c.cv('eps'))
    recip(p, rstd[:], tmp[:], [tmp], [rstd])
    for kc in range(KC):
        stt(p, xn_out_fn(kc), xt[:, kc, :], gcols[:, kc:kc + 1], rstd[:], ALU.mult, ALU.mult,
            [xt, rstd, c.cvb], xn_bufs)


def load_xt(p, xt, src_d, t0, n=512):
    v = src_d.rearrange("(kc q) t -> q kc t", q=128)
    for q in range(4):
        p.dma("sp", xt[:, q * 4:(q + 1) * 4, :], v[:, q * 4:(q + 1) * 4, t0:t0 + n], writes=[xt])


def phase_inproj(p, c, L, x_d):
    S, NB, T = c.S, c.NB, c.T
    w_v = c.w_in[L].rearrange("(kc q) n -> q kc n", q=128)
    blocks = [
        (0, 512, "F", c.qk_pre, 0, None), (512, 512, "F", c.qk_pre, 512, None),
        (1024, 512, "T", c.mv_tok, 0, None),
        (1536, 512, "F", c.soT, 0, "sigmoid"),
        (2048, 8, "T", c.mg_tok, 0, None),
        (2056, 512, "T", c.sz_tok, 0, "silu"), (2568, 512, "T", c.sz_tok, 512, "silu"),
        (3080, 512, "F", c.xbc_pre, 0, None), (3592, 512, "F", c.xbc_pre, 512, None),
        (4104, 512, "F", c.xbc_pre, 1024, None),
        (4616, 16, "T", c.sdt_tok, 0, None),
        (4632, 512, "F", c.aqT, 0, "qscale"), (5144, 512, "F", c.akT, 0, None),
        (5656, 512, "T", c.av_tok, 0, None),
    ]
    with p.scope():
        xn = p.sbuf([128, KC, S], BF16, "xn")
        xt = [p.sbuf([128, KC, 512], F32, "xt") for _ in range(2)]
        sq = p.sbuf([128, KC, 512], BF16, "sq")
        tmp = p.sbuf([128, 512], F32, "tmp")
        rstd = p.sbuf([128, 512], F32, "rstd")
        wb = [p.sbuf([128, KC, 512], BF16, "wb") for _ in range(2)]
        st32 = [p.sbuf([128, 512], F32, "st32") for _ in range(2)]
        st16 = [p.sbuf([128, 512], BF16, "st16") for _ in range(2)]
        pss = [p.psum([128, 512], F32, "psA") for _ in range(6)]
        psn = p.psum([128, 512], F32, "psn")
        gcols = c.cv(f'g_mix{L}')
        si = 0
        pi = 0
        for b in range(NB):
            for tt_ in range(S // 512):
                t0 = b * S + tt_ * 512
                x = xt[tt_ % 2]
                load_xt(p, x, x_d, t0)
                norm_tile(p, c, x, gcols, lambda kc, tt_=tt_: xn[:, kc, tt_ * 512:(tt_ + 1) * 512],
                          sq, psn, tmp, rstd, [xn])
            for bi, (c0, w, kind, dest, d0, post) in enumerate(blocks):
                wbuf = wb[bi % 2]
                for q in range(4):
                    p.dma("pool", wbuf[:, q * 4:(q + 1) * 4, 0:w], w_v[:, q * 4:(q + 1) * 4, c0:c0 + w],
                          writes=[wbuf])
                is16 = dest.dtype == BF16
                for tt_ in range(S // 512):
                    for sub in range(4):
                        ps = pss[pi % 6]
                        pi += 1
                        stg = (st16 if is16 else st32)[si % 2]
                        si += 1
                        if kind == "F":
                            if sub * 128 >= w:
                                continue
                            for kc in range(KC):
                                mm(p, ps[:], wbuf[:, kc, sub * 128:(sub + 1) * 128],
                                   xn[:, kc, tt_ * 512:(tt_ + 1) * 512], kc == 0, kc == KC - 1,
                                   [wbuf, xn], [ps])
                            if post == "sigmoid":
                                act(p, stg[:], ps[:], AF.Sigmoid, [ps], [stg])
                            elif post == "qscale":
                                act(p, stg[:], ps[:], AF.Copy, [ps], [stg], scale=128 ** -0.5)
                            elif si % 2:
                                act(p, stg[:], ps[:], AF.Copy, [ps], [stg])
                            else:
                                cp(p, stg[:], ps[:], [ps], [stg])
                            r0 = d0 + sub * 128
                            p.dma("sp", dest[r0:r0 + 128, b * S + tt_ * 512: b * S + (tt_ + 1) * 512], stg[:],
                                  reads=[stg])
                        else:
                            tk = tt_ * 512 + sub * 128
                            for kc in range(KC):
                                mm(p, ps[:, 0:w], xn[:, kc, tk:tk + 128], wbuf[:, kc, 0:w],
                                   kc == 0, kc == KC - 1, [wbuf, xn], [ps])
                            if post == "silu":
                                act(p, stg[:, 0:w], ps[:, 0:w], AF.Silu, [ps], [stg])
                            elif si % 2:
                                act(p, stg[:, 0:w], ps[:, 0:w], AF.Copy, [ps], [stg])
                            else:
                                cp(p, stg[:, 0:w], ps[:, 0:w], [ps], [stg])
                            p.dma("sp", dest[b * S + tk: b * S + tk + 128, d0:d0 + w], stg[:, 0:w], reads=[stg])


def phase_conv(p, c, L):
    S, NB = c.S, c.NB
    with p.scope():
        xin = [p.sbuf([128, S + 3], F32, "cin") for _ in range(2)]
        acc = [p.sbuf([128, S], F32, "cacc") for _ in range(2)]
        o16 = [p.sbuf([128, S], BF16, "co16") for _ in range(2)]
        for x in xin:
            mset(p, x[:, 0:3], 0.0, [x])
        i = 0
        for (src, dst, nch, wc, bc, kscale_from) in (
                (c.qk_pre, c.qkT, 8, c.cv(f'mcw{L}'), c.cv(f'mcb{L}'), 4), (c.xbc_pre, c.xbcT, 12, c.cv(f'scw{L}'), c.cv(f'scb{L}'), 99)):
            for ch in range(nch):
                for b in range(NB):
                    x = xin[i % 2]
                    a = acc[i % 2]
                    o = o16[i % 2]
                    i += 1
                    p.dma("sp", x[:, 3:3 + S], src[ch * 128:(ch + 1) * 128, b * S:(b + 1) * S], writes=[x])
                    ts(p, a[:], x[:, 0:S], wc[:, ch * 4:ch * 4 + 1], bc[:, ch:ch + 1], ALU.mult, ALU.add,
                       [x, c.cvb], [a])
                    for j in range(1, 4):
                        stt(p, a[:], x[:, j:j + S], wc[:, ch * 4 + j:ch * 4 + j + 1], a[:], ALU.mult, ALU.add,
                            [x, a, c.cvb], [a])
                    if ch >= kscale_from:
                        act(p, a[:], a[:], AF.Silu, [a], [a])
                        ts(p, o[:], a[:], 128 ** -0.5, None, ALU.mult, None, [a], [o], eng="pool")
                    else:
                        act(p, o[:], a[:], AF.Silu, [a], [o])
                    p.dma("sp", dst[ch * 128:(ch + 1) * 128, b * S:(b + 1) * S], o[:], reads=[o])


def phase_mlstm(p, c, L):
    S, NB = c.S, c.NB
    NCH = S // 128
    cv = c.cv
    with p.scope():
        qT = p.sbuf([128, 4, S], BF16, "qT")
        kT = p.sbuf([128, 4, S], BF16, "kT")
        v = p.sbuf([128, NCH, 512], BF16, "v")
        so = p.sbuf([128, 4, S], F32, "so")
        g = p.sbuf([128, NCH, 8], F32, "g")
        li = p.sbuf([128, NCH, 4], F32, "li")
        lf = p.sbuf([128, NCH, 4], F32, "lf")
        hT = p.sbuf([128, 4, S], BF16, "hT")
        CT = p.sbuf([128, 4, 128], F32, "CT")
        CTb = p.sbuf([128, 4, 128], BF16, "CTb")
        nb_ = p.sbuf([128, 4, 128], F32, "nb")
        nbb = p.sbuf([128, 4, 128], BF16, "nbb")
        R_ = [p.sbuf([128, 512], F32, "R") for _ in range(2)]
        WT_ = [p.sbuf([128, 512], F32, "WT") for _ in range(2)]
        E_ = [p.sbuf([128, 512], F32, "E") for _ in range(2)]
        ST_ = [p.sbuf([128, 512], BF16, "ST") for _ in range(2)]
        qs_ = [p.sbuf([128, 4, 128], BF16, "qs") for _ in range(2)]
        colb_ = [p.sbuf([128, 4], F32, "colb") for _ in range(2)]
        a__ = [p.sbuf([128, 4], F32, "a") for _ in range(2)]
        dn_ = [p.sbuf([128, 512], F32, "dn") for _ in range(2)]
        hr_ = [p.sbuf([128, 512], F32, "hr") for _ in range(2)]
        sq_ = [p.sbuf([128, 512], BF16, "sq") for _ in range(2)]
        t1_ = [p.sbuf([128, 512], F32, "t1") for _ in range(2)]
        ka_ = [p.sbuf([128, 4, 128], BF16, "ka") for _ in range(2)]
        psA = p.psum([128, 512], F32, "psA")
        psB = p.psum([128, 512], F32, "psB")
        psC = p.psum([128, 512], F32, "psC")
        psD = p.psum([128, 512], F32, "psD")
        psE = p.psum([128, 512], F32, "psE")
        psG = p.psum([128, 1024], BF16, "psG")
        psH1 = p.psum([128, 512], F32, "psH1")
        psH2 = p.psum([128, 512], F32, "psH2")
        for b in range(NB):
            tb = b * S
            for h in range(4):
                p.dma("sp", qT[:, h, :], c.qkT[h * 128:(h + 1) * 128, tb:tb + S], writes=[qT])
                p.dma("sp", kT[:, h, :], c.qkT[512 + h * 128:512 + (h + 1) * 128, tb:tb + S], writes=[kT])
                p.dma("sp", so[:, h, :], c.soT[h * 128:(h + 1) * 128, tb:tb + S], writes=[so])
            p.dma("sp", v[:], c.mv_tok[tb:tb + S, :].rearrange("(n q) f -> q n f", q=128), writes=[v])
            p.dma("sp", g[:], c.mg_tok[tb:tb + S, :].rearrange("(n q) f -> q n f", q=128), writes=[g])
            gb = cv(f"mgb{L}")
            tt(p, li[:], g[:, :, 0:4], gb[:, 0:4].unsqueeze(1).to_broadcast([128, NCH, 4]), ALU.add,
               [g, c.cvb], [li])
            tt(p, lf[:], g[:, :, 4:8], gb[:, 4:8].unsqueeze(1).to_broadcast([128, NCH, 4]), ALU.add,
               [g, c.cvb], [lf])
            act(p, lf[:], lf[:], AF.Exp, [lf], [lf], scale=-1.0)
            act(p, lf[:], lf[:], AF.Ln, [lf], [lf], bias=1.0)
            ts(p, lf[:], lf[:], -1.0, None, ALU.mult, None, [lf], [lf])
            for st_ in (CT, nb_):
                mset(p, st_[:], 0.0, [st_])
            for st_ in (CTb, nbb):
                mset(p, st_[:], 0.0, [st_])
            for ch in range(NCH):
                t0 = ch * 128
                sl = slice(t0, t0 + 128)
                pr = ch % 2
                R, WT, E, ST, qs, colb, a_ = R_[pr], WT_[pr], E_[pr], ST_[pr], qs_[pr], colb_[pr], a__[pr]
                dn, hr, sq, t1, ka = dn_[pr], hr_[pr], sq_[pr], t1_[pr], ka_[pr]
                tt(p, R[:].rearrange("q (h j) -> q h j", h=4), c.tri.unsqueeze(1).to_broadcast([128, 4, 128]),
                   lf[:, ch, :].unsqueeze(2).to_broadcast([128, 4, 128]), ALU.mult, [lf, c.cf], [R])
                mm(p, psA[:], c.ones_f, R[:], True, True, [R, c.cf], [psA])
                mm(p, psB[:, 0:4], c.tri, lf[:, ch, :], True, True, [lf, c.cf], [psB])
                tt(p, colb[:], li[:, ch, :], psB[:, 0:4], ALU.subtract, [li, psB], [colb])
                for h in range(4):
                    act(p, WT[:, h * 128:(h + 1) * 128], psA[:, h * 128:(h + 1) * 128], AF.Exp,
                        [psA, colb], [WT], bias=colb[:, h:h + 1])
                act(p, E[:], psA[:], AF.Exp, [psA], [E])
                for h in range(4):
                    act(p, a_[:, h:h + 1], psA[:, h * 128 + 127:h * 128 + 128], AF.Exp, [psA, colb], [a_],
                        bias=colb[:, h:h + 1])
                tt(p, WT[:], WT[:], c.mask4, ALU.mult, [WT, c.cf], [WT])
                for h in range(4):
                    mm(p, psC[:, h * 128:(h + 1) * 128], kT[:, h, sl], qT[:, h, sl], True, True,
                       [kT, qT], [psC])
                tt(p, ST[:], psC[:], WT[:], ALU.mult, [psC, WT], [ST])
                tt(p, qs[:], qT[:, :, sl], E[:].rearrange("q (h j) -> q h j", h=4), ALU.mult, [qT, E], [qs])
                for h in range(4):
                    hs = slice(h * 128, (h + 1) * 128)
                    mm(p, psD[:, hs], v[:, ch, hs], ST[:, hs], True, False, [v, ST], [psD])
                    mm(p, psD[:, hs], CTb[:, h, :], qs[:, h, :], False, True, [CTb, qs], [psD])
                    mm(p, psE[:, hs], c.ones_bf, ST[:, hs], True, False, [ST, c.cb], [psE])
                    mm(p, psE[:, hs], nbb[:, h, :], qs[:, h, :], False, True, [nbb, qs], [psE])
                act(p, dn[:], psE[:], AF.Abs, [psE], [dn])
                ts(p, dn[:], dn[:], 1.0, None, ALU.max, None, [dn], [dn])
                recip(p, dn[:], dn[:], [dn], [dn])
                tt(p, hr[:], psD[:], dn[:], ALU.mult, [psD, dn], [hr])
                act(p, sq[:], hr[:], AF.Square, [hr], [sq])
                mm(p, psB[:], c.ones_bf, sq[:], True, True, [sq, c.cb], [psB])
                act(p, t1[:], psB[:], AF.Sqrt, [psB, c.cvb], [t1], scale=1.0 / 128, bias=cv("eps"))
                recip(p, t1[:], t1[:], [t1], [t1])
                tt(p, hr[:], hr[:], t1[:], ALU.mult, [hr, t1], [hr])
                ng = cv(f"mng{L}")
                for h in range(4):
                    stt(p, hT[:, h, sl], hr[:, h * 128:(h + 1) * 128], ng[:, h:h + 1], so[:, h, sl],
                        ALU.mult, ALU.mult, [hr, so, c.cvb], [hT])
                for h in range(4):
                    tr(p, psG[:, h * 128:(h + 1) * 128], kT[:, h, sl], c.ident_bf, [kT, c.cb], [psG])
                for h in range(4):
                    ts(p, ka[:, h, :], psG[:, h * 128:(h + 1) * 128], a_[:, h:h + 1], None, ALU.mult, None,
                       [psG, a_], [ka])
                for h in range(4):
                    hs = slice(h * 128, (h + 1) * 128)
                    mm(p, psH1[:, hs], ka[:, h, :], v[:, ch, hs], True, True, [ka, v], [psH1])
                    mm(p, psH2[:, hs], ka[:, h, :], c.ones_bf, True, True, [ka, c.cb], [psH2])
                for h in range(4):
                    hs = slice(h * 128, (h + 1) * 128)
                    dcol = E[:, h * 128 + 127:h * 128 + 128]
                    stt(p, CT[:, h, :], CT[:, h, :], dcol, psH1[:, hs], ALU.mult, ALU.add, [CT, E, psH1], [CT])
                    stt(p, nb_[:, h, :], nb_[:, h, :], dcol, psH2[:, hs], ALU.mult, ALU.add, [nb_, E, psH2], [nb_])
                cp(p, CTb[:], CT[:], [CT], [CTb], eng="pool")
                cp(p, nbb[:], nb_[:], [nb_], [nbb], eng="pool")
            for h in range(4):
                p.dma("sp", c.mixT[h * 128:(h + 1) * 128, tb:tb + S], hT[:, h, :], reads=[hT])


def phase_ssd(p, c, L):
    S, NB = c.S, c.NB
    NCH = S // 128
    cv = c.cv
    with p.scope():
        xsT = p.sbuf([128, 8, S], BF16, "xsT")
        BT = p.sbuf([128, 2, S], BF16, "BT")
        CTm = p.sbuf([128, 2, S], BF16, "CTm")
        dtr = p.sbuf([128, NCH, 16], F32, "dtr")
        dt = p.sbuf([128, NCH, 16], F32, "dt")
        tmpd = p.sbuf([128, NCH, 16], F32, "tmpd")
        Aneg = p.sbuf([128, 16], F32, "Aneg")
        sng = p.sbuf([128, 1024], F32, "sng")
        yT = p.sbuf([128, 8, S], BF16, "yT")
        szb = [p.sbuf([128, 1024], F32, "sz") for _ in range(2)]
        hst = p.sbuf([128, 1024], F32, "hst")
        hstb = p.sbuf([128, 1024], BF16, "hstb")
        a_c_ = [p.sbuf([128, 16], F32, "a_c") for _ in range(2)]
        R2_ = [p.sbuf([128, 2048], F32, "R2") for _ in range(2)]
        acol_ = [p.sbuf([128, 16], F32, "acol") for _ in range(2)]
        tot_ = [p.sbuf([128, 16], F32, "tot") for _ in range(2)]
        Dm_ = [p.sbuf([128, 2048], F32, "Dm") for _ in range(2)]
        CBm_ = [p.sbuf([128, 256], F32, "CBm") for _ in range(2)]
        W_ = [p.sbuf([128, 2048], BF16, "W") for _ in range(2)]
        Xdt_ = [p.sbuf([128, 1024], BF16, "Xdt") for _ in range(2)]
        Xd_ = [p.sbuf([128, 1024], F32, "Xd") for _ in range(2)]
        Btok = p.sbuf([128, 256], BF16, "Btok")
        eac = p.sbuf([128, 16], F32, "eac")
        dte = p.sbuf([128, 16], F32, "dte")
        ecd = p.sbuf([128, 16], F32, "ecd")
        y = p.sbuf([128, 1024], F32, "y")
        ysq = p.sbuf([128, 1024], F32, "ysq")
        ss = p.sbuf([128, 2], F32, "ss")
        yn = p.sbuf([128, 1024], BF16, "yn")
        Xdd = p.sbuf([128, 1024], BF16, "Xdd")
        pb = [p.psum([128, 512], F32, f"pb{i}") for i in range(4)]
        pm = p.psum([128, 512], F32, "pm")
        px = p.psum([128, 1024], BF16, "px")
        py0 = p.psum([128, 512], F32, "py0")
        py1 = p.psum([128, 512], F32, "py1")
        p.dma("sp", sng[:], c.sng_d[L], writes=[sng])
        act(p, Aneg[:], cv(f"salog{L}"), AF.Exp, [c.cvb], [Aneg])
        ts(p, Aneg[:], Aneg[:], -1.0, None, ALU.mult, None, [Aneg], [Aneg])
        dsk = cv(f"sd{L}")
        for b in range(NB):
            tb = b * S
            for ch8 in range(8):
                p.dma("sp", xsT[:, ch8, :], c.xbcT[ch8 * 128:(ch8 + 1) * 128, tb:tb + S], writes=[xsT])
            for g_ in range(2):
                p.dma("sp", BT[:, g_, :], c.xbcT[1024 + g_ * 128:1024 + (g_ + 1) * 128, tb:tb + S], writes=[BT])
                p.dma("sp", CTm[:, g_, :], c.xbcT[1280 + g_ * 128:1280 + (g_ + 1) * 128, tb:tb + S], writes=[CTm])
            p.dma("sp", dtr[:], c.sdt_tok[tb:tb + S, :].rearrange("(n q) f -> q n f", q=128), writes=[dtr])
            tt(p, dtr[:], dtr[:], cv(f"sdtb{L}").unsqueeze(1).to_broadcast([128, NCH, 16]), ALU.add,
               [dtr, c.cvb], [dtr])
            act(p, tmpd[:], dtr[:], AF.Abs, [dtr], [tmpd])
            act(p, tmpd[:], tmpd[:], AF.Exp, [tmpd], [tmpd], scale=-1.0)
            act(p, tmpd[:], tmpd[:], AF.Ln, [tmpd], [tmpd], bias=1.0)
            stt(p, dt[:], dtr[:], 0.0, tmpd[:], ALU.max, ALU.add, [dtr, tmpd], [dt])
            mset(p, hst[:], 0.0, [hst])
            mset(p, hstb[:], 0.0, [hstb])
            for ch in range(NCH):
                t0 = ch * 128
                sl = slice(t0, t0 + 128)
                sz = szb[ch % 2]
                pr = ch % 2
                a_c, R2, acol, tot, Dm, CBm, W, Xdt, Xd = (a_c_[pr], R2_[pr], acol_[pr], tot_[pr], Dm_[pr],
                                                           CBm_[pr], W_[pr], Xdt_[pr], Xd_[pr])
                p.dma("sp", sz[:], c.sz_tok[tb + t0:tb + t0 + 128, :], writes=[sz])
                tt(p, a_c[:], dt[:, ch, :], Aneg[:], ALU.mult, [dt, Aneg], [a_c])
                for hq in range(2):
                    tt(p, R2[:, hq * 1024:(hq + 1) * 1024].rearrange("q (h j) -> q h j", h=8),
                       c.tri.unsqueeze(1).to_broadcast([128, 8, 128]),
                       a_c[:, hq * 8:(hq + 1) * 8].unsqueeze(2).to_broadcast([128, 8, 128]), ALU.mult,
                       [a_c, c.cf], [R2], eng=("dve" if hq else "pool"))
                for q in range(4):
                    mm(p, pb[q][:], c.ones_f, R2[:, q * 512:(q + 1) * 512], True, True, [R2, c.cf], [pb[q]])
                mm(p, pm[:, 0:16], c.tri, a_c[:], True, True, [a_c, c.cf], [pm])
                for g_ in range(2):
                    mm(p, pm[:, 128 + g_ * 128:256 + g_ * 128], BT[:, g_, sl], CTm[:, g_, sl], True, True,
                       [BT, CTm], [pm])
                cp(p, acol[:], pm[:, 0:16], [pm], [acol])
                tt(p, CBm[:], pm[:, 128:384], c.mask4[:, 0:256], ALU.mult, [pm, c.cf], [CBm])
                for q in range(4):
                    cp(p, tot[:, q * 4:(q + 1) * 4],
                       pb[q][:].rearrange("q (h j) -> q h j", h=4)[:, :, 127], [pb[q]], [tot])
                for h in range(16):
                    q, hh = h // 4, h % 4
                    ts(p, Dm[:, h * 128:(h + 1) * 128], pb[q][:, hh * 128:(hh + 1) * 128], acol[:, h:h + 1], 0.0,
                       ALU.subtract, ALU.min, [pb[q], acol], [Dm])
                act(p, Dm[:], Dm[:], AF.Exp, [Dm], [Dm])
                for g_ in range(2):
                    tt(p, W[:, g_ * 1024:(g_ + 1) * 1024].rearrange("q (h j) -> q h j", h=8),
                       Dm[:, g_ * 1024:(g_ + 1) * 1024].rearrange("q (h j) -> q h j", h=8),
                       CBm[:, g_ * 128:(g_ + 1) * 128].unsqueeze(1).to_broadcast([128, 8, 128]), ALU.mult,
                       [Dm, CBm], [W])
                for ch8 in range(8):
                    tr(p, px[:, ch8 * 128:(ch8 + 1) * 128], xsT[:, ch8, sl], c.ident_bf, [xsT, c.cb], [px])
                tt(p, Xdt[:].rearrange("q (h e) -> q h e", h=16), px[:].rearrange("q (h e) -> q h e", h=16),
                   dt[:, ch, :].unsqueeze(2).to_broadcast([128, 16, 64]), ALU.mult, [px, dt], [Xdt])
                tt(p, Xd[:].rearrange("q (h e) -> q h e", h=16), px[:].rearrange("q (h e) -> q h e", h=16),
                   dsk.unsqueeze(2).to_broadcast([128, 16, 64]), ALU.mult, [px, c.cvb], [Xd])
                for h in range(16):
                    py = py0 if h < 8 else py1
                    hh = h % 8
                    mm(p, py[:, hh * 64:(hh + 1) * 64], W[:, h * 128:(h + 1) * 128], Xdt[:, h * 64:(h + 1) * 64],
                       True, True, [W, Xdt], [py])
                for g_ in range(2):
                    mm(p, pb[g_][:], CTm[:, g_, sl], hstb[:, g_ * 512:(g_ + 1) * 512], True, True,
                       [CTm, hstb], [pb[g_]])
                act(p, eac[:], acol[:], AF.Exp, [acol], [eac])
                for g_ in range(2):
                    tt(p, y[:, g_ * 512:(g_ + 1) * 512].rearrange("q (h e) -> q h e", h=8),
                       pb[g_][:].rearrange("q (h e) -> q h e", h=8),
                       eac[:, g_ * 8:(g_ + 1) * 8].unsqueeze(2).to_broadcast([128, 8, 64]), ALU.mult,
                       [pb[g_], eac], [y])
                tt(p, y[:, 0:512], y[:, 0:512], py0[:], ALU.add, [y, py0], [y])
                tt(p, y[:, 512:1024], y[:, 512:1024], py1[:], ALU.add, [y, py1], [y])
                tt(p, y[:], y[:], Xd[:], ALU.add, [y, Xd], [y])
                tt(p, y[:], y[:], sz[:], ALU.mult, [y, sz], [y])
                for g_ in range(2):
                    act(p, ysq[:, g_ * 512:(g_ + 1) * 512], y[:, g_ * 512:(g_ + 1) * 512], AF.Square, [y], [ysq, ss],
                        accum_out=ss[:, g_:g_ + 1])
                act(p, ss[:], ss[:], AF.Sqrt, [ss, c.cvb], [ss], scale=1.0 / 512, bias=cv("eps"))
                recip(p, ss[:], ss[:], [ss], [ss])
                for g_ in range(2):
                    stt(p, yn[:, g_ * 512:(g_ + 1) * 512], y[:, g_ * 512:(g_ + 1) * 512], ss[:, g_:g_ + 1],
                        sng[:, g_ * 512:(g_ + 1) * 512], ALU.mult, ALU.mult, [y, ss, sng], [yn])
                for ch8 in range(8):
                    tr(p, px[:, ch8 * 128:(ch8 + 1) * 128], yn[:, ch8 * 128:(ch8 + 1) * 128], c.ident_bf,
                       [yn, c.cb], [px])
                cp(p, yT[:, :, sl], px[:].rearrange("q (c j) -> q c j", c=8), [px], [yT], eng="act")
                tt(p, dte[:], tot[:], acol[:], ALU.subtract, [tot, acol], [dte])
                act(p, dte[:], dte[:], AF.Exp, [dte], [dte])
                act(p, ecd[:], tot[:], AF.Exp, [tot], [ecd])
                tt(p, Xdd[:].rearrange("q (h e) -> q h e", h=16), Xdt[:].rearrange("q (h e) -> q h e", h=16),
                   dte[:].unsqueeze(2).to_broadcast([128, 16, 64]), ALU.mult, [Xdt, dte], [Xdd])
                for g_ in range(2):
                    tr(p, px[:, g_ * 128:(g_ + 1) * 128], BT[:, g_, sl], c.ident_bf, [BT, c.cb], [px])
                cp(p, Btok[:], px[:, 0:256], [px], [Btok])
                for g_ in range(2):
                    mm(p, pb[2 + g_][:], Btok[:, g_ * 128:(g_ + 1) * 128], Xdd[:, g_ * 512:(g_ + 1) * 512],
                       True, True, [Btok, Xdd], [pb[2 + g_]])
                tt(p, hst[:].rearrange("q (h e) -> q h e", h=16), hst[:].rearrange("q (h e) -> q h e", h=16),
                   ecd[:].unsqueeze(2).to_broadcast([128, 16, 64]), ALU.mult, [hst, ecd], [hst])
                for g_ in range(2):
                    tt(p, hst[:, g_ * 512:(g_ + 1) * 512], hst[:, g_ * 512:(g_ + 1) * 512], pb[2 + g_][:], ALU.add,
                       [hst, pb[2 + g_]], [hst])
                cp(p, hstb[:], hst[:], [hst], [hstb], eng="pool")
            for ch8 in range(8):
                p.dma("sp", c.mixT[512 + ch8 * 128:512 + (ch8 + 1) * 128, tb:tb + S], yT[:, ch8, :], reads=[yT])


def phase_moba(p, c, L):
    S, NB = c.S, c.NB
    NQT = S // 128
    NBLK = S // 256
    with p.scope():
        qT = p.sbuf([128, 4, S], BF16, "aq")
        kT = p.sbuf([128, 4, S], BF16, "ak")
        v = p.sbuf([128, NQT, 512], BF16, "av")
        oT = p.sbuf([128, 4, S], BF16, "oT")
        km = p.sbuf([128, 8], F32, "km")
        kmb = p.sbuf([128, 8], BF16, "kmb")
        sc = p.sbuf([128, 8], F32, "sc")
        top8 = p.sbuf([128, 8], F32, "top8")
        brow = p.sbuf([128, 8], F32, "brow")
        biasT = p.sbuf([8, S], BF16, "biasT")
        scA = p.sbuf([128, 128], F32, "scA")
        topA = p.sbuf([128, 128], F32, "topA")
        browA = p.sbuf([128, 128], F32, "browA")
        PT = [p.sbuf([128, 512], BF16, "PT") for _ in range(2)]
        rden = p.sbuf([128, 512], F32, "rden")
        pS = [p.psum([128, 512], F32, f"pS{i}") for i in range(2)]
        pO = p.psum([128, 512], F32, "pO")
        pD = p.psum([128, 512], F32, "pD")
        pq = p.psum([128, 512], F32, "pq")
        pt = p.psum([128, 512], F32, "ptr")
        sel8 = c.cbv("sel8", 8)
        mr = c.cbv("mr")
        k = 0
        for b in range(NB):
            tb = b * S
            for h in range(4):
                p.dma("sp", qT[:, h, :], c.aqT[h * 128:(h + 1) * 128, tb:tb + S], writes=[qT])
                p.dma("sp", kT[:, h, :], c.akT[h * 128:(h + 1) * 128, tb:tb + S], writes=[kT])
            p.dma("sp", v[:], c.av_tok[tb:tb + S, :].rearrange("(n q) f -> q n f", q=128), writes=[v])
            for h in range(4):
                mset(p, km[:], 0.0, [km])
                p.op("dve", lambda e, h=h: e.tensor_reduce(out=km[:, 0:NBLK],
                                                          in_=kT[:, h, :].rearrange("q (n j) -> q n j", j=256),
                                                          axis=AX.X, op=ALU.add), [kT], [km])
                ts(p, kmb[:], km[:], 1.0 / 256, None, ALU.mult, None, [km], [kmb])
                for qt in range(NQT):
                    mm(p, pq[:, qt * 8:(qt + 1) * 8], qT[:, h, qt * 128:(qt + 1) * 128], kmb[:], True, True,
                       [qT, kmb], [pq])
                NC8 = NQT * 8
                tt(p, scA[:, 0:NC8], pq[:, 0:NC8], c.cfv("vmask")[:, 0:NC8], ALU.add, [pq, c.cf], [scA])
                for qt in range(NQT):
                    p.op("dve", lambda e, qt=qt: e.max(out=topA[:, qt * 8:(qt + 1) * 8], in_=scA[:, qt * 8:(qt + 1) * 8]),
                         [scA], [topA])
                tt(p, browA[:, 0:NC8].rearrange("q (t n) -> q t n", n=8),
                   scA[:, 0:NC8].rearrange("q (t n) -> q t n", n=8),
                   topA[:, 0:NC8].rearrange("q (t n) -> q t n", n=8)[:, :, 2:3].to_broadcast([128, NQT, 8]),
                   ALU.is_ge, [scA, topA], [browA])
                ts(p, browA[:, 0:NC8], browA[:, 0:NC8], -NEG, NEG, ALU.mult, ALU.add, [browA], [browA])
                tt(p, browA[:, 0:NC8], browA[:, 0:NC8], c.cfv("ownmask")[:, 0:NC8], ALU.mult, [browA, c.cf], [browA])
                for q4 in range(NQT // 4):
                    for j in range(4):
                        qt = q4 * 4 + j
                        tr(p, pt[0:8, j * 128:(j + 1) * 128], browA[:, qt * 8:(qt + 1) * 8], c.ident_f,
                           [browA, c.cf], [pt])
                    cp(p, biasT[:, q4 * 512:(q4 + 1) * 512], pt[0:8, :], [pt], [biasT], eng="act")
                for jt in range(S // 512):
                    js = slice(jt * 512, (jt + 1) * 512)
                    nst = 4 * jt + 4
                    def emit_S(st, kk):
                        ps = pS[kk % 2]
                        diag = st >= 4 * jt
                        mm(p, ps[:], kT[:, h, st * 128:(st + 1) * 128], qT[:, h, js], True, False, [kT, qT], [ps])
                        mm(p, ps[:], sel8[:, (st // 2) * 128:(st // 2 + 1) * 128], biasT[:, js], False, not diag,
                           [biasT, c.cb], [ps])
                        if diag:
                            r = st - 4 * jt
                            mm(p, ps[:], c.ident_bf, mr[:, r * 512:(r + 1) * 512], False, True, [c.cb], [ps])
                    emit_S(0, k)
                    for st in range(nst):
                        ps = pS[k % 2]
                        pt_ = PT[k % 2]
                        if st + 1 < nst:
                            emit_S(st + 1, k + 1)
                        act(p, pt_[:], ps[:], AF.Exp, [ps], [pt_])
                        mm(p, pO[:], v[:, st, h * 128:(h + 1) * 128], pt_[:], st == 0, st == nst - 1, [v, pt_], [pO])
                        mm(p, pD[:], c.ones_bf, pt_[:], st == 0, st == nst - 1, [pt_, c.cb], [pD])
                        k += 1
                    recip(p, rden[:], pD[:], [pD], [rden])
                    tt(p, oT[:, h, js], pO[:], rden[:], ALU.mult, [pO, rden], [oT])
            for h in range(4):
                p.dma("sp", c.mixT[1536 + h * 128:1536 + (h + 1) * 128, tb:tb + S], oT[:, h, :], reads=[oT])


def load_w_cast(p, wbuf_ap, src_ap, wbuf, nsplit=4, axis_len=None):
    n = axis_len
    step = max(1, n // nsplit)
    for q0 in range(0, n, step):
        q1 = min(n, q0 + step)
        p.dma("pool", wbuf_ap[:, q0:q1, :], src_ap[:, q0:q1, :], writes=[wbuf])


def store_xt(p, dst_d, xt, t0, n=512):
    v = dst_d.rearrange("(kc q) t -> q kc t", q=128)
    for q in range(4):
        p.dma("sp", v[:, q * 4:(q + 1) * 4, t0:t0 + n], xt[:, q * 4:(q + 1) * 4, :], reads=[xt])


def phase_outproj(p, c, L, x_in, x_out):
    T = c.T
    with p.scope():
        wo = p.sbuf([128, KC, D], BF16, "wo")
        load_w_cast(p, wo, c.w_out[L].rearrange("(kc q) n -> q kc n", q=128), wo, 8, KC)
        mt = [p.sbuf([128, KC, 512], BF16, "mt") for _ in range(2)]
        xt = [p.sbuf([128, KC, 512], F32, "xt") for _ in range(2)]
        pss = [p.psum([128, 512], F32, f"po{i}") for i in range(4)]
        mv = c.mixT.rearrange("(kc q) t -> q kc t", q=128)
        for ti in range(T // 512):
            t0 = ti * 512
            m = mt[ti % 2]
            x = xt[ti % 2]
            for q in range(2):
                p.dma("sp", m[:, q * 8:(q + 1) * 8, :], mv[:, q * 8:(q + 1) * 8, t0:t0 + 512], writes=[m])
            load_xt(p, x, x_in, t0)
            for dc in range(KC):
                ps = pss[dc % 4]
                for kc in range(KC):
                    mm(p, ps[:], wo[:, kc, dc * 128:(dc + 1) * 128], m[:, kc, :], kc == 0, kc == KC - 1, [wo, m], [ps])
                tt(p, x[:, dc, :], x[:, dc, :], ps[:], ALU.add, [x, ps], [x])
            store_xt(p, x_out, x, t0)


def phase_ffn(p, c, x_in, x_out, moe):
    T = c.T
    L = 1 if moe else 0
    NF = 56 if moe else 44
    NE = 8 if moe else 1
    NS = 2 if T % 1024 == 0 else 1
    NG = 4
    GF = NF // NG
    TT = NS * 512
    with p.scope():
        xt = [p.sbuf([128, KC, 512], F32, "xt") for _ in range(NS)]
        xn = [p.sbuf([128, KC, 512], BF16, "xn") for _ in range(NS)]
        hraw = p.sbuf([128, 8192], F32, "hraw")
        hT = hraw.t[:, 0:NS * GF * 256].bitcast(BF16).rearrange("q (s f t) -> q s f t", s=NS, t=512)
        sq = hraw.t[:, 0:4096].bitcast(BF16).rearrange("q (f t) -> q f t", t=512)
        xn32 = hraw.t[:, 0:8192].rearrange("q (f t) -> q f t", t=512)
        tmp = p.sbuf([128, 512], F32, "tmp")
        rstd = p.sbuf([128, 512], F32, "rstd")
        gsb = p.sbuf([128, 512], F32, "gsb")
        BW = 2 if GF % 2 == 0 else 1
        wg = [p.sbuf([128, KC, 128 * BW], BF16, "wg") for _ in range(2)]
        wu = [p.sbuf([128, KC, 128 * BW], BF16, "wu") for _ in range(2)]
        wd = [p.sbuf([128, GF, 256], BF16, "wd") for _ in range(2)]
        pg = [p.psum([128, 512], F32, f"pg{i}") for i in range(2)]
        pu = [p.psum([128, 512], F32, f"pu{i}") for i in range(2)]
        pd = [p.psum([128, 512], F32, f"pd{i}") for i in range(2)]
        pn = p.psum([128, 512], F32, "pn")
        px = p.psum([128, 512], F32, "pxx")
        if moe:
            wr = p.sbuf([128, KC, 8], F32, "wr")
            p.dma("sp", wr[:], c.moe_r[0].rearrange("(kc q) n -> q kc n", q=128), writes=[wr])
            lg = p.sbuf([128, 4, 8], F32, "lg")
            top8 = p.sbuf([128, 8], F32, "top8")
            cmb = p.sbuf([128, 4, 8], F32, "cmb")
            ssum = p.sbuf([128, 1], F32, "ssum")
            nmx = p.sbuf([128, 1], F32, "nmx")
            combT = [p.sbuf([8, 512], F32, "combT") for _ in range(NS)]
            cbe = [p.sbuf([128, 512], F32, "cbe") for _ in range(NS)]
            sel8f = c.cfv("sel8", 8)
        gcols = c.cv(f"g_ffn{L}")
        wi = 0
        di = 0
        pi = 0
        for ti in range(T // TT):
            for s_ in range(NS):
                t0 = ti * TT + s_ * 512
                x = xt[s_]
                load_xt(p, x, x_in, t0)
                act(p, sq, x[:], AF.Square, [x], [hraw])
                for kc in range(KC):
                    mm(p, pn[:], c.ones_bf, sq[:, kc, :], kc == 0, kc == KC - 1, [hraw, c.cb], [pn])
                act(p, tmp[:], pn[:], AF.Sqrt, [pn, c.cvb], [tmp], scale=1.0 / D, bias=c.cv("eps"))
                recip(p, rstd[:], tmp[:], [tmp], [rstd])
                for kc in range(KC):
                    stt(p, xn[s_][:, kc, :], x[:, kc, :], gcols[:, kc:kc + 1], rstd[:], ALU.mult, ALU.mult,
                        [x, rstd, c.cvb], [xn[s_]])
                if moe:
                    for kc in range(KC):
                        stt(p, xn32[:, kc, :], x[:, kc, :], gcols[:, kc:kc + 1], rstd[:], ALU.mult, ALU.mult,
                            [x, rstd, c.cvb], [hraw])
                    for sub in range(4):
                        for kc in range(KC):
                            mm(p, px[:, sub * 8:(sub + 1) * 8], xn32[:, kc, sub * 128:(sub + 1) * 128], wr[:, kc, :],
                               kc == 0, kc == KC - 1, [hraw, wr], [px])
                    cp(p, lg[:], px[:, 0:32].rearrange("q (s e) -> q s e", e=8), [px], [lg])
                    for sub in range(4):
                        p.op("dve", lambda e, sub=sub: e.max(out=top8[:], in_=lg[:, sub, :]), [lg], [top8])
                        ts(p, nmx[:], top8[:, 0:1], -1.0, None, ALU.mult, None, [top8], [nmx])
                        act(p, cmb[:, sub, :], lg[:, sub, :], AF.Exp, [lg, nmx], [cmb], bias=nmx[:, 0:1])
                        stt(p, cmb[:, sub, :], lg[:, sub, :], top8[:, 1:2], cmb[:, sub, :], ALU.is_ge, ALU.mult,
                            [lg, top8, cmb], [cmb])
                        p.op("dve", lambda e, sub=sub: e.tensor_reduce(out=ssum[:], in_=cmb[:, sub, :], axis=AX.X,
                                                                      op=ALU.add), [cmb], [ssum])
                        recip(p, ssum[:], ssum[:], [ssum], [ssum])
                        ts(p, cmb[:, sub, :], cmb[:, sub, :], ssum[:, 0:1], None, ALU.mult, None, [cmb, ssum], [cmb])
                        tr(p, pn[0:8, sub * 128:(sub + 1) * 128], cmb[:, sub, :], c.ident_f, [cmb, c.cf], [pn])
                    cp(p, combT[s_][:], pn[0:8, :], [pn], [combT[s_]])
            for e_ in range(NE):
                if moe:
                    wgv = c.moe_wg[0, e_].rearrange("(kc q) n -> q kc n", q=128)
                    wuv = c.moe_wu[0, e_].rearrange("(kc q) n -> q kc n", q=128)
                    wdv = c.moe_wd[0, e_].rearrange("(f q) n -> q f n", q=128)
                    for s_ in range(NS):
                        mm(p, pn[:], sel8f[:, e_ * 128:(e_ + 1) * 128], combT[s_][:], True, True, [combT[s_], c.cf], [pn])
                        cp(p, cbe[s_][:], pn[:], [pn], [cbe[s_]], eng="act")
                else:
                    wgv = c.ffn_wg[0].rearrange("(kc q) n -> q kc n", q=128)
                    wuv = c.ffn_wu[0].rearrange("(kc q) n -> q kc n", q=128)
                    wdv = c.ffn_wd[0].rearrange("(f q) n -> q f n", q=128)
                for gi in range(NG):
                    for fp in range(GF // BW):
                        fc0 = gi * GF + fp * BW
                        g_, u_ = wg[wi % 2], wu[wi % 2]
                        wi += 1
                        load_w_cast(p, g_, wgv[:, :, fc0 * 128:(fc0 + BW) * 128], g_, 2, KC)
                        load_w_cast(p, u_, wuv[:, :, fc0 * 128:(fc0 + BW) * 128], u_, 2, KC)
                        for s_ in range(NS):
                            for j in range(BW):
                                fl = fp * BW + j
                                js = slice(j * 128, (j + 1) * 128)
                                a, b2 = pg[pi % 2], pu[pi % 2]
                                pi += 1
                                for kc in range(KC):
                                    mm(p, a[:], g_[:, kc, js], xn[s_][:, kc, :], kc == 0, kc == KC - 1,
                                       [g_, xn[s_]], [a])
                                for kc in range(KC):
                                    mm(p, b2[:], u_[:, kc, js], xn[s_][:, kc, :], kc == 0, kc == KC - 1,
                                       [u_, xn[s_]], [b2])
                                act(p, gsb[:], a[:], AF.Silu, [a], [gsb])
                                if moe:
                                    tt(p, gsb[:], gsb[:], cbe[s_][:], ALU.mult, [gsb, cbe[s_]], [gsb])
                                tt(p, hT[:, s_, fl, :], gsb[:], b2[:], ALU.mult, [gsb, b2], [hraw])
                    for dp in range(KC // 2):
                        d_ = wd[di % 2]
                        di += 1
                        load_w_cast(p, d_, wdv[:, gi * GF:(gi + 1) * GF, dp * 256:(dp + 1) * 256], d_, 2, GF)
                        for s_ in range(NS):
                            for j in range(2):
                                dc = dp * 2 + j
                                o = pd[pi % 2]
                                pi += 1
                                for fl in range(GF):
                                    mm(p, o[:], d_[:, fl, j * 128:(j + 1) * 128], hT[:, s_, fl, :], fl == 0,
                                       fl == GF - 1, [d_, hraw], [o])
                                tt(p, xt[s_][:, dc, :], xt[s_][:, dc, :], o[:], ALU.add, [xt[s_], o], [xt[s_]])
            for s_ in range(NS):
                store_xt(p, x_out, xt[s_], ti * TT + s_ * 512)


def phase_ple(p, c, L, x_in, x_out, final):
    T = c.T
    with p.scope():
        wgt = p.sbuf([128, KC, D], BF16, "wgt")
        wp = p.sbuf([128, 2, D], BF16, "wp")
        load_w_cast(p, wgt, c.ple_gate[L].rearrange("(kc q) n -> q kc n", q=128), wgt, 8, KC)
        load_w_cast(p, wp, c.ple_proj[L].rearrange("(kc q) n -> q kc n", q=128), wp, 1, 2)
        xt = [p.sbuf([128, KC, 512], F32, "xt") for _ in range(2)]
        xn = p.sbuf([128, KC, 512], BF16, "xn")
        sq = p.sbuf([128, KC, 512], BF16, "sq")
        pt = [p.sbuf([128, 2, 512], BF16, "pt") for _ in range(2)]
        tmp = p.sbuf([128, 512], F32, "tmp")
        rstd = p.sbuf([128, 512], F32, "rstd")
        sg = p.sbuf([128, 512], F32, "sg")
        pn = p.psum([128, 512], F32, "pn")
        pgs = [p.psum([128, 512], F32, f"pg{i}") for i in range(2)]
        pps = [p.psum([128, 512], F32, f"pp{i}") for i in range(2)]
        gcols = c.cv(f"g_ple{L}")
        pv = c.pT[L].rearrange("(kc q) t -> q kc t", q=128)
        for ti in range(T // 512):
            t0 = ti * 512
            x = xt[ti % 2]
            pp_ = pt[ti % 2]
            load_xt(p, x, x_in, t0)
            p.dma("pool", pp_[:], pv[:, :, t0:t0 + 512], writes=[pp_])
            norm_tile(p, c, x, gcols, lambda kc: xn[:, kc, :], sq, pn, tmp, rstd, [xn])
            for dc in range(KC):
                a, b2 = pgs[dc % 2], pps[dc % 2]
                for kc in range(KC):
                    mm(p, a[:], wgt[:, kc, dc * 128:(dc + 1) * 128], xn[:, kc, :], kc == 0, kc == KC - 1, [wgt, xn], [a])
                for kc in range(2):
                    mm(p, b2[:], wp[:, kc, dc * 128:(dc + 1) * 128], pp_[:, kc, :], kc == 0, kc == 1, [wp, pp_], [b2])
                act(p, sg[:], a[:], AF.Sigmoid, [a], [sg])
                tt(p, sg[:], sg[:], b2[:], ALU.mult, [sg, b2], [sg])
                tt(p, x[:, dc, :], x[:, dc, :], sg[:], ALU.add, [x, sg], [x], eng="pool")
            if final:
                act(p, sq[:], x[:], AF.Square, [x], [sq])
                for kc in range(KC):
                    mm(p, pn[:], c.ones_bf, sq[:, kc, :], kc == 0, kc == KC - 1, [sq, c.cb], [pn])
                act(p, tmp[:], pn[:], AF.Sqrt, [pn, c.cvb], [tmp], scale=1.0 / D, bias=c.cv("eps"))
                recip(p, rstd[:], tmp[:], [tmp], [rstd])
                gf = c.cv("g_fin")
                for kc in range(KC):
                    stt(p, x[:, kc, :], x[:, kc, :], gf[:, kc:kc + 1], rstd[:], ALU.mult, ALU.mult,
                        [x, rstd, c.cvb], [x])
            store_xt(p, x_out, x, t0)


class Layout:
    def __init__(self):
        self.off = {}
        self.w = 0

    def add(self, name, width):
        self.off[name] = (self.w, width)
        self.w += width


def cv_layout():
    l = Layout()
    for L in range(2):
        for nm, w in (("g_mix", 16), ("g_ffn", 16), ("g_ple", 16), ("mcw", 32), ("mcb", 8), ("scw", 48),
                      ("scb", 12), ("mng", 4), ("mgb", 8), ("sdtb", 16), ("salog", 16), ("sd", 16)):
            l.add(f"{nm}{L}", w)
    l.add("g_fin", 16)
    l.add("eps", 1)
    return l


def cf_layout():
    l = Layout()
    for nm, w in (("ones", 128), ("ident", 128), ("tri", 128), ("mask4", 512), ("sel8", 1024),
                  ("vmask", 128), ("ownmask", 128)):
        l.add(nm, w)
    return l


def cb_layout():
    l = Layout()
    for nm, w in (("ones", 128), ("ident", 128), ("mr", 2048), ("sel8", 1024)):
        l.add(nm, w)
    return l


CVL, CFL, CBL = cv_layout(), cf_layout(), cb_layout()


def cols(v, n):
    return np.ascontiguousarray(np.asarray(v, np.float32).reshape(n, 128).T)


def pack_consts(inp):
    cv = np.zeros((128, CVL.w), np.float32)

    def put(name, arr):
        o, w = CVL.off[name]
        cv[:, o:o + w] = arr
    for L in range(2):
        put(f"g_mix{L}", cols(inp["ln_mix"][L], 16))
        put(f"g_ffn{L}", cols(inp["ln_ffn"][L], 16))
        put(f"g_ple{L}", cols(inp["ln_ple"][L], 16))
        w = np.asarray(inp["m_conv_w"][L], np.float32)
        put(f"mcw{L}", w.T.reshape(8, 128, 4).transpose(1, 0, 2).reshape(128, 32))
        put(f"mcb{L}", cols(inp["m_conv_b"][L], 8))
        w = np.asarray(inp["s_conv_w"][L], np.float32)
        put(f"scw{L}", w.T.reshape(12, 128, 4).transpose(1, 0, 2).reshape(128, 48))
        put(f"scb{L}", cols(inp["s_conv_b"][L], 12))
        put(f"mng{L}", cols(inp["m_norm_g"][L], 4))
        put(f"mgb{L}", np.broadcast_to(np.asarray(inp["m_gate_b"][L], np.float32)[None, :], (128, 8)))
        put(f"sdtb{L}", np.broadcast_to(np.asarray(inp["s_dt_bias"][L], np.float32)[None, :], (128, 16)))
        put(f"salog{L}", np.broadcast_to(np.asarray(inp["s_a_log"][L], np.float32)[None, :], (128, 16)))
        put(f"sd{L}", np.broadcast_to(np.asarray(inp["s_d"][L], np.float32)[None, :], (128, 16)))
    put("g_fin", cols(inp["ln_final"], 16))
    put("eps", np.full((128, 1), EPS, np.float32))

    cf = np.zeros((128, CFL.w), np.float32)
    tri = (np.arange(128)[:, None] <= np.arange(128)[None, :]).astype(np.float32)
    sel8 = np.zeros((128, 8, 128), np.float32)
    for k in range(8):
        sel8[k, k, :] = 1.0
    qbv = np.arange(16)[:, None] // 2
    nv = np.arange(8)[None, :]
    vmask = np.where(nv < qbv, 0.0, -1e30).astype(np.float32).reshape(1, 128)
    ownmask = np.where(nv == qbv, 0.0, 1.0).astype(np.float32).reshape(1, 128)
    for nm, arr in (("ones", np.ones((128, 128))), ("ident", np.eye(128)), ("tri", tri),
                    ("mask4", np.tile(tri, (1, 4))), ("sel8", sel8.reshape(128, 1024)),
                    ("vmask", np.broadcast_to(vmask, (128, 128))), ("ownmask", np.broadcast_to(ownmask, (128, 128)))):
        o, w = CFL.off[nm]
        cf[:, o:o + w] = arr
    cbf = np.zeros((128, CBL.w), np.float32)
    mr = np.zeros((128, 4, 512), np.float32)
    for r in range(4):
        mr[:, r, :] = np.where(r * 128 + np.arange(128)[:, None] > np.arange(512)[None, :], NEG, 0.0)
    for nm, arr in (("ones", np.ones((128, 128))), ("ident", np.eye(128)), ("mr", mr.reshape(128, 2048)),
                    ("sel8", sel8.reshape(128, 1024))):
        o, w = CBL.off[nm]
        cbf[:, o:o + w] = arr
    return cv, cf, cbf.astype(ml_dtypes.bfloat16)


def build(S=2048, NB=2, stages=("all",), depth=2, dbg=()):
    T = NB * S
    nc = bass.Bass("TRN2", target_bir_lowering=False)
    c = Ctx()
    c.S, c.NB, c.T = S, NB, T

    c.declared = []

    def din(name, shape, dt=F32, need=True):
        if not need:
            return None
        c.declared.append(name)
        return nc.dram_tensor(name, list(shape), dt, kind="ExternalInput").ap()
    stages = set(stages)
    allst = "all" in stages
    nffn = allst or "ffn" in stages
    nmoe = (allst or "moe" in stages) and depth > 1
    nple = allst or "ple" in stages

    def dscr(name, shape, dt):
        if name in dbg:
            return nc.dram_tensor(name, list(shape), dt, kind="ExternalOutput").ap()
        return nc.dram_tensor(name, list(shape), dt).ap()

    xT = din("xT", [D, T])
    c.pT = din("pT", [2, 256, T])
    c.w_in = din("w_in", [2, D, DPROJ])
    c.w_out = din("w_out", [2, D, D])
    c.ffn_wg = din("ffn_w_gate", [1, D, 5632], need=nffn)
    c.ffn_wu = din("ffn_w_up", [1, D, 5632], need=nffn)
    c.ffn_wd = din("ffn_w_down", [1, 5632, D], need=nffn)
    c.moe_r = din("moe_router", [1, D, 8], need=nmoe)
    c.moe_wg = din("moe_w_gate", [1, 8, D, 7168], need=nmoe)
    c.moe_wu = din("moe_w_up", [1, 8, D, 7168], need=nmoe)
    c.moe_wd = din("moe_w_down", [1, 8, 7168, D], need=nmoe)
    c.ple_proj = din("ple_proj", [2, 256, D], need=nple)
    c.ple_gate = din("ple_gate", [2, D, D], need=nple)
    c.sng_d = din("sng", [2, 128, 1024])
    cvec = din("cvec", [128, CVL.w])
    cf32 = din("cf32", [128, CFL.w])
    cbf = din("cbf", [128, CBL.w], BF16)
    yT = nc.dram_tensor("yT", [D, T], F32, kind="ExternalOutput").ap()

    c.qk_pre = dscr("qk_pre", [1024, T], F32)
    c.qkT = dscr("qkT", [1024, T], BF16)
    c.mv_tok = dscr("mv_tok", [T, 512], BF16)
    c.soT = dscr("soT", [512, T], F32)
    c.mg_tok = dscr("mg_tok", [T, 8], F32)
    c.sz_tok = dscr("sz_tok", [T, 1024], F32)
    c.xbc_pre = dscr("xbc_pre", [1536, T], F32)
    c.xbcT = dscr("xbcT", [1536, T], BF16)
    c.sdt_tok = dscr("sdt_tok", [T, 16], F32)
    c.aqT = dscr("aqT", [512, T], BF16)
    c.akT = dscr("akT", [512, T], BF16)
    c.av_tok = dscr("av_tok", [T, 512], BF16)
    c.mixT = dscr("mixT", [D, T], BF16)
    c.xs = [dscr("xA", [D, T], F32), dscr("xB", [D, T], F32), dscr("xC", [D, T], F32)]

    p = Prog(nc)
    c.cvb = p.sbuf([128, CVL.w], F32, "cvec")
    c.cf = p.sbuf([128, CFL.w], F32, "cf32")
    c.cb = p.sbuf([128, CBL.w], BF16, "cbf")
    p.dma("sp", c.cvb[:], cvec, writes=[c.cvb])
    p.dma("sp", c.cf[:], cf32, writes=[c.cf])
    p.dma("sp", c.cb[:], cbf, writes=[c.cb])

    def cv(name):
        o, w = CVL.off[name]
        return c.cvb.t[:, o:o + w]

    def cfv(name, np_=128):
        o, w = CFL.off[name]
        return c.cf.t[0:np_, o:o + w]

    def cbv(name, np_=128):
        o, w = CBL.off[name]
        return c.cb.t[0:np_, o:o + w]
    c.cv, c.cfv, c.cbv = cv, cfv, cbv
    c.ones_bf = cbv("ones")
    c.ident_bf = cbv("ident")
    c.ones_f = cfv("ones")
    c.ident_f = cfv("ident")
    c.tri = cfv("tri")
    c.mask4 = cfv("mask4")

    x_cur = xT
    for L in range(depth):
        if allst or "inproj" in stages:
            phase_inproj(p, c, L, x_cur)
        if allst or "conv" in stages:
            phase_conv(p, c, L)
        if allst or "mlstm" in stages:
            phase_mlstm(p, c, L)
        if allst or "ssd" in stages:
            phase_ssd(p, c, L)
        if allst or "moba" in stages:
            phase_moba(p, c, L)
        if allst or "outproj" in stages:
            phase_outproj(p, c, L, x_cur, c.xs[0])
            x_cur = c.xs[0]
        if allst or "ffn" in stages:
            phase_ffn(p, c, x_cur, c.xs[1], moe=(L == 1))
            x_cur = c.xs[1]
        if allst or "ple" in stages:
            last = (L == depth - 1)
            phase_ple(p, c, L, x_cur, yT if last else c.xs[2], final=last)
            x_cur = c.xs[2]
    p.finish()
    nc._declared_inputs = list(c.declared)
    return nc


def host_inputs(inputs, S, NB, ncores, nc=None):
    cv, cf, cbf = pack_consts(inputs)
    x = np.asarray(inputs["x"], np.float32)
    pp = np.asarray(inputs["p"], np.float32)
    shared = {"cvec": cv, "cf32": cf, "cbf": cbf,
              "sng": np.ascontiguousarray(np.broadcast_to(np.asarray(inputs["s_norm_g"], np.float32)[:, None, :], (2, 128, 1024)))}
    for k in ("w_in", "w_out", "ffn_w_gate", "ffn_w_up", "ffn_w_down", "moe_router", "moe_w_gate",
              "moe_w_up", "moe_w_down", "ple_proj", "ple_gate"):
        shared[k] = np.ascontiguousarray(np.asarray(inputs[k], np.float32))
    maps = []
    for i in range(ncores):
        xb = x[i * NB:(i + 1) * NB].reshape(NB * S, D)
        m = dict(shared)
        m["xT"] = np.ascontiguousarray(xb.T)
        m["pT"] = np.ascontiguousarray(pp[:, i * NB:(i + 1) * NB].reshape(2, NB * S, 256).transpose(0, 2, 1))
        if nc is not None:
            m = {k: v for k, v in m.items() if k in nc._declared_inputs}
        maps.append(m)
    return maps


def kernel(**inputs):
    S, NB, ncores = 2048, 2, 8
    nc = build(S, NB)
    maps = host_inputs(inputs, S, NB, ncores, nc)
    res = run_bass_kernel_spmd(nc, maps, core_ids=list(range(ncores)))
    outs = [np.asarray(r["yT"]).T.reshape(NB, S, D) for r in res.results]
    return np.concatenate(outs, axis=0).astype(np.float32)
```

```python
from contextlib import ExitStack
import numpy as np
import ml_dtypes
import concourse.bass as bass
import concourse.mybir as mybir
from concourse.bass_utils import run_bass_kernel_spmd
from concourse.alu_op_type import AluOpType as ALU

F32 = mybir.dt.float32
BF16 = mybir.dt.bfloat16
AF = mybir.ActivationFunctionType
AX = mybir.AxisListType

N_DMA_SEMS = 12
D = 2048
KC = 16
DPROJ = 6168
EPS = 1e-6
NEG = -30000.0


class Buf:
    __slots__ = ("t", "name", "writers", "readers", "gen_war")

    def __init__(self, t, name=""):
        self.t = t
        self.name = name
        self.writers = []
        self.readers = []
        self.gen_war = []

    def __getitem__(self, k):
        return self.t[k]


class Eng:
    def __init__(self, name):
        self.name = name
        self.sem = None
        self.count = 0
        self.ops = []
        self.waited = {}
        self.dma_sems = []
        self.dma_vals = []
        self.dma_i = 0


class Prog:
    def __init__(self, nc):
        self.nc = nc
        self.es = ExitStack()
        self.root_es = self.es
        self.engs = {n: Eng(n) for n in ("pe", "act", "dve", "pool", "sp")}
        for n, e in self.engs.items():
            e.sem = self.es.enter_context(nc.semaphore("c_" + n))
        for n in ("sp", "pool", "act"):
            e = self.engs[n]
            for i in range(N_DMA_SEMS):
                e.dma_sems.append(self.es.enter_context(nc.semaphore(f"d_{n}{i}")))
                e.dma_vals.append(0)
        self.all_dma_events = []
        self.nbuf = 0

    def sbuf(self, shape, dtype, name=None):
        self.nbuf += 1
        name = (name or "sb") + f"_{self.nbuf}"
        t = self.es.enter_context(self.nc.sbuf_tensor(name, list(shape), dtype))
        return Buf(t, name)

    def psum(self, shape, dtype, name=None):
        self.nbuf += 1
        name = (name or "ps") + f"_{self.nbuf}"
        t = self.es.enter_context(self.nc.psum_tensor(name, list(shape), dtype))
        return Buf(t, name)

    @staticmethod
    def _continuing(engname, b):
        return (engname.startswith("dma_") and b.writers and not b.readers
                and all(w[0].startswith("dma_") for w in b.writers))

    def _deps(self, engname, reads, writes):
        deps = {}
        is_dma = engname.startswith("dma_")

        def add(ev, raw):
            en, sem, val = ev
            if en == engname and not is_dma:
                if not raw or engname == "pe":
                    return
            k = id(sem)
            if k not in deps or deps[k][1] < val:
                deps[k] = (sem, val)
        for b in reads:
            for w in b.writers:
                add(w, True)
        for b in writes:
            if self._continuing(engname, b):
                for w in b.gen_war:
                    add(w, False)
            else:
                for w in b.writers:
                    add(w, False)
                for r in b.readers:
                    add(r, False)
        return deps

    def _emit_waits(self, eng, deps):
        waits = []
        for k, (sem, val) in deps.items():
            if eng.waited.get(k, 0) >= val:
                continue
            eng.waited[k] = val
            waits.append((sem, val))
        return waits

    def _record(self, engname, ev, reads, writes):
        for b in writes:
            if self._continuing(engname, b):
                b.writers.append(ev)
            else:
                b.gen_war = b.writers + b.readers
                b.writers = [ev]
                b.readers = []
        for b in reads:
            if not engname.startswith("dma_"):
                b.readers = [r for r in b.readers if r[0] != engname]
            b.readers.append(ev)

    def op(self, engname, fn, reads=(), writes=()):
        eng = self.engs[engname]
        waits = self._emit_waits(eng, self._deps(engname, reads, writes))
        eng.count += 1
        val = eng.count
        sem = eng.sem

        def run(e, waits=waits, fn=fn, sem=sem):
            for s, v in waits:
                e.wait_ge(s, v)
            fn(e).then_inc(sem, 1)
        eng.ops.append(run)
        ev = (engname, sem, val)
        self._record(engname, ev, reads, writes)
        return ev

    def dma(self, qname, out, in_, reads=(), writes=(), **kw):
        eng = self.engs[qname]
        deps = self._deps("dma_" + qname, reads, writes)
        i = eng.dma_i % N_DMA_SEMS
        eng.dma_i += 1
        sem = eng.dma_sems[i]
        prev = eng.dma_vals[i]
        if prev:
            k = id(sem)
            if k not in deps or deps[k][1] < prev:
                deps[k] = (sem, prev)
        waits = self._emit_waits(eng, deps)
        val = prev + 16
        eng.dma_vals[i] = val

        def run(e, waits=waits, sem=sem, out=out, in_=in_, kw=kw):
            for s, v in waits:
                e.wait_ge(s, v)
            e.dma_start(out=out, in_=in_, **kw).then_inc(sem, 16)
        eng.ops.append(run)
        ev = ("dma_" + qname, sem, val)
        self._record("dma_" + qname, ev, reads, writes)
        self.all_dma_events.append((sem, val))
        return ev

    def _all_events(self):
        evs = {}
        for sem, val in self.all_dma_events:
            k = id(sem)
            if k not in evs or evs[k][1] < val:
                evs[k] = (sem, val)
        self.all_dma_events = list(evs.values())
        for n, e in self.engs.items():
            if e.count:
                evs[id(e.sem)] = (e.sem, e.count)
        return evs

    def barrier(self):
        evs = self._all_events()
        for n, e in self.engs.items():
            waits = self._emit_waits(e, {k: v for k, v in evs.items() if k != id(e.sem)})
            if waits:
                def run(en, waits=waits):
                    for s, v in waits:
                        en.wait_ge(s, v)
                e.ops.append(run)

    def flush(self):
        nc = self.nc
        engs = self.engs
        with nc.Block() as block:
            @block.tensor
            def _(e):
                for f in engs["pe"].ops:
                    f(e)

            @block.scalar
            def _(e):
                for f in engs["act"].ops:
                    f(e)

            @block.vector
            def _(e):
                for f in engs["dve"].ops:
                    f(e)

            @block.gpsimd
            def _(e):
                for f in engs["pool"].ops:
                    f(e)

            @block.sync
            def _(e):
                for f in engs["sp"].ops:
                    f(e)
        for e in engs.values():
            e.ops = []

    def scope(self):
        prog = self

        class _S:
            def __enter__(s):
                s.saved = prog.es
                prog.es = ExitStack()
                return prog

            def __exit__(s, *a):
                if a[0] is None:
                    prog.barrier()
                    prog.flush()
                prog.es.close()
                prog.es = s.saved
                return False
        return _S()

    def finish(self):
        self.barrier()
        self.flush()
        self.es.close()


def mm(p, out, lhsT, rhs, start, stop, reads, writes):
    p.op("pe", lambda e: e.matmul(out, lhsT, rhs, start=start, stop=stop), reads, writes)


def tr(p, out, in_, ident, reads, writes):
    p.op("pe", lambda e: e.transpose(out, in_, ident), reads, writes)


def act(p, out, in_, func, reads, writes, **kw):
    p.op("act", lambda e: e.activation(out=out, in_=in_, func=func, **kw), reads, writes)


def tt(p, out, in0, in1, op, reads, writes, eng="dve"):
    p.op(eng, lambda e: e.tensor_tensor(out=out, in0=in0, in1=in1, op=op), reads, writes)


def ts(p, out, in0, s1, s2, op0, op1, reads, writes, eng="dve"):
    if op1 is None:
        p.op(eng, lambda e: e.tensor_scalar(out=out, in0=in0, scalar1=s1, scalar2=None, op0=op0), reads, writes)
    else:
        p.op(eng, lambda e: e.tensor_scalar(out=out, in0=in0, scalar1=s1, scalar2=s2, op0=op0, op1=op1), reads, writes)


def stt(p, out, in0, scalar, in1, op0, op1, reads, writes):
    p.op("dve", lambda e: e.scalar_tensor_tensor(out=out, in0=in0, scalar=scalar, in1=in1, op0=op0, op1=op1),
         reads, writes)


def cp(p, out, in_, reads, writes, eng="dve"):
    if eng == "act":
        p.op(eng, lambda e: e.activation(out=out, in_=in_, func=AF.Copy), reads, writes)
    else:
        p.op(eng, lambda e: e.tensor_copy(out=out, in_=in_), reads, writes)


def recip(p, out, in_, reads, writes):
    p.op("dve", lambda e: e.reciprocal(out=out, in_=in_), reads, writes)


def mset(p, ap, val, writes, eng="dve"):
    p.op(eng, lambda e: e.memset(ap, val), (), writes)


class Ctx:
    pass


def norm_tile(p, c, xt, gcols, xn_out_fn, sq, ps, tmp, rstd, xn_bufs):
    act(p, sq[:], xt[:], AF.Square, [xt], [sq])
    for kc in range(KC):
        mm(p, ps[:], c.ones_bf, sq[:, kc, :], kc == 0, kc == KC - 1, [sq, c.cb], [ps])
    act(p, tmp[:], ps[:], AF.Sqrt, [ps, c.cvb], [tmp], scale=1.0 / D, bias=c.cv('eps'))
    recip(p, rstd[:], tmp[:], [tmp], [rstd])
    for kc in range(KC):
        stt(p, xn_out_fn(kc), xt[:, kc, :], gcols[:, kc:kc + 1], rstd[:], ALU.mult, ALU.mult,
            [xt, rstd, c.cvb], xn_bufs)


def load_xt(p, xt, src_d, t0, n=512):
    v = src_d.rearrange("(kc q) t -> q kc t", q=128)
    for q in range(4):
        p.dma("sp", xt[:, q * 4:(q + 1) * 4, :], v[:, q * 4:(q + 1) * 4, t0:t0 + n], writes=[xt])


def phase_inproj(p, c, L, x_d):
    S, NB, T = c.S, c.NB, c.T
    w_v = c.w_in[L].rearrange("(kc q) n -> q kc n", q=128)
    blocks = [
        (0, 512, "F", c.qk_pre, 0, None), (512, 512, "F", c.qk_pre, 512, None),
        (1024, 512, "T", c.mv_tok, 0, None),
        (1536, 512, "F", c.soT, 0, "sigmoid"),
        (2048, 8, "T", c.mg_tok, 0, None),
        (2056, 512, "T", c.sz_tok, 0, "silu"), (2568, 512, "T", c.sz_tok, 512, "silu"),
        (3080, 512, "F", c.xbc_pre, 0, None), (3592, 512, "F", c.xbc_pre, 512, None),
        (4104, 512, "F", c.xbc_pre, 1024, None),
        (4616, 16, "T", c.sdt_tok, 0, None),
        (4632, 512, "F", c.aqT, 0, "qscale"), (5144, 512, "F", c.akT, 0, None),
        (5656, 512, "T", c.av_tok, 0, None),
    ]
    with p.scope():
        xn = p.sbuf([128, KC, S], BF16, "xn")
        xt = [p.sbuf([128, KC, 512], F32, "xt") for _ in range(2)]
        sq = p.sbuf([128, KC, 512], BF16, "sq")
        tmp = p.sbuf([128, 512], F32, "tmp")
        rstd = p.sbuf([128, 512], F32, "rstd")
        wb = [p.sbuf([128, KC, 512], BF16, "wb") for _ in range(2)]
        st32 = [p.sbuf([128, 512], F32, "st32") for _ in range(3)]
        st16 = [p.sbuf([128, 512], BF16, "st16") for _ in range(3)]
        pss = [p.psum([128, 512], F32, "psA") for _ in range(6)]
        psn = p.psum([128, 512], F32, "psn")
        gcols = c.cv(f'g_mix{L}')
        si = 0
        pi = 0
        for b in range(NB):
            for tt_ in range(S // 512):
                t0 = b * S + tt_ * 512
                x = xt[tt_ % 2]
                load_xt(p, x, x_d, t0)
                norm_tile(p, c, x, gcols, lambda kc, tt_=tt_: xn[:, kc, tt_ * 512:(tt_ + 1) * 512],
                          sq, psn, tmp, rstd, [xn])
            for bi, (c0, w, kind, dest, d0, post) in enumerate(blocks):
                wbuf = wb[bi % 2]
                for q in range(4):
                    p.dma("pool", wbuf[:, q * 4:(q + 1) * 4, 0:w], w_v[:, q * 4:(q + 1) * 4, c0:c0 + w],
                          writes=[wbuf])
                is16 = dest.dtype == BF16
                for tt_ in range(S // 512):
                    for sub in range(4):
                        ps = pss[pi % 6]
                        pi += 1
                        stg = (st16 if is16 else st32)[si % 3]
                        si += 1
                        if kind == "F":
                            if sub * 128 >= w:
                                continue
                            for kc in range(KC):
                                mm(p, ps[:], wbuf[:, kc, sub * 128:(sub + 1) * 128],
                                   xn[:, kc, tt_ * 512:(tt_ + 1) * 512], kc == 0, kc == KC - 1,
                                   [wbuf, xn], [ps])
                            if post == "sigmoid":
                                act(p, stg[:], ps[:], AF.Sigmoid, [ps], [stg])
                            elif post == "qscale":
                                act(p, stg[:], ps[:], AF.Copy, [ps], [stg], scale=128 ** -0.5)
                            elif si % 2:
                                act(p, stg[:], ps[:], AF.Copy, [ps], [stg])
                            else:
                                cp(p, stg[:], ps[:], [ps], [stg])
                            r0 = d0 + sub * 128
                            p.dma("sp", dest[r0:r0 + 128, b * S + tt_ * 512: b * S + (tt_ + 1) * 512], stg[:],
                                  reads=[stg])
                        else:
                            tk = tt_ * 512 + sub * 128
                            for kc in range(KC):
                                mm(p, ps[:, 0:w], xn[:, kc, tk:tk + 128], wbuf[:, kc, 0:w],
                                   kc == 0, kc == KC - 1, [wbuf, xn], [ps])
                            if post == "silu":
                                act(p, stg[:, 0:w], ps[:, 0:w], AF.Silu, [ps], [stg])
                            elif si % 2:
                                act(p, stg[:, 0:w], ps[:, 0:w], AF.Copy, [ps], [stg])
                            else:
                                cp(p, stg[:, 0:w], ps[:, 0:w], [ps], [stg])
                            p.dma("sp", dest[b * S + tk: b * S + tk + 128, d0:d0 + w], stg[:, 0:w], reads=[stg])


def phase_conv(p, c, L):
    S, NB = c.S, c.NB
    with p.scope():
        xin = [p.sbuf([128, S + 3], F32, "cin") for _ in range(2)]
        acc = [p.sbuf([128, S], F32, "cacc") for _ in range(2)]
        o16 = [p.sbuf([128, S], BF16, "co16") for _ in range(2)]
        for x in xin:
            mset(p, x[:, 0:3], 0.0, [x])
        jobs = []
        for (src, dst, nch, wc, bc, kscale_from) in (
                (c.qk_pre, c.qkT, 8, c.cv(f'mcw{L}'), c.cv(f'mcb{L}'), 4), (c.xbc_pre, c.xbcT, 12, c.cv(f'scw{L}'), c.cv(f'scb{L}'), 99)):
            for ch in range(nch):
                for b in range(NB):
                    jobs.append((src, dst, wc, bc, kscale_from, ch, b))

        def issue_load(i):
            src, dst, wc, bc, kscale_from, ch, b = jobs[i]
            x = xin[i % 2]
            p.dma("sp", x[:, 3:3 + S], src[ch * 128:(ch + 1) * 128, b * S:(b + 1) * S], writes=[x])
        issue_load(0)
        for i, (src, dst, wc, bc, kscale_from, ch, b) in enumerate(jobs):
            x = xin[i % 2]
            a = acc[i % 2]
            o = o16[i % 2]
            if i + 1 < len(jobs):
                issue_load(i + 1)
            ts(p, a[:], x[:, 0:S], wc[:, ch * 4:ch * 4 + 1], bc[:, ch:ch + 1], ALU.mult, ALU.add,
               [x, c.cvb], [a])
            for j in range(1, 4):
                stt(p, a[:], x[:, j:j + S], wc[:, ch * 4 + j:ch * 4 + j + 1], a[:], ALU.mult, ALU.add,
                    [x, a, c.cvb], [a])
            if ch >= kscale_from:
                act(p, a[:], a[:], AF.Silu, [a], [a])
                ts(p, o[:], a[:], 128 ** -0.5, None, ALU.mult, None, [a], [o], eng="pool")
            else:
                act(p, o[:], a[:], AF.Silu, [a], [o])
            p.dma("sp", dst[ch * 128:(ch + 1) * 128, b * S:(b + 1) * S], o[:], reads=[o])


def phase_mlstm(p, c, L):
    S, NB = c.S, c.NB
    NCH = S // 128
    cv = c.cv
    with p.scope():
        qT = p.sbuf([128, 4, S], BF16, "qT")
        kT = p.sbuf([128, 4, S], BF16, "kT")
        v = p.sbuf([128, NCH, 512], BF16, "v")
        so = p.sbuf([128, 4, S], F32, "so")
        g = p.sbuf([128, NCH, 8], F32, "g")
        li = p.sbuf([128, NCH, 4], F32, "li")
        lf = p.sbuf([128, NCH, 4], F32, "lf")
        hT = p.sbuf([128, 4, S], BF16, "hT")
        CT = p.sbuf([128, 4, 128], F32, "CT")
        CTb = p.sbuf([128, 4, 128], BF16, "CTb")
        nb_ = p.sbuf([128, 4, 128], F32, "nb")
        nbb = p.sbuf([128, 4, 128], BF16, "nbb")
        R_ = [p.sbuf([128, 512], F32, "R") for _ in range(2)]
        WT_ = [p.sbuf([128, 512], F32, "WT") for _ in range(2)]
        E_ = [p.sbuf([128, 512], F32, "E") for _ in range(2)]
        ST_ = [p.sbuf([128, 512], BF16, "ST") for _ in range(2)]
        qs_ = [p.sbuf([128, 4, 128], BF16, "qs") for _ in range(2)]
        colb_ = [p.sbuf([128, 4], F32, "colb") for _ in range(2)]
        a__ = [p.sbuf([128, 4], F32, "a") for _ in range(2)]
        dn_ = [p.sbuf([128, 512], F32, "dn") for _ in range(2)]
        hr_ = [p.sbuf([128, 512], F32, "hr") for _ in range(2)]
        sq_ = [p.sbuf([128, 512], BF16, "sq") for _ in range(2)]
        t1_ = [p.sbuf([128, 512], F32, "t1") for _ in range(2)]
        ka_ = [p.sbuf([128, 4, 128], BF16, "ka") for _ in range(2)]
        psA = p.psum([128, 512], F32, "psA")
        psB = p.psum([128, 512], F32, "psB")
        psC = p.psum([128, 512], F32, "psC")
        psD = p.psum([128, 512], F32, "psD")
        psE = p.psum([128, 512], F32, "psE")
        psG = p.psum([128, 1024], BF16, "psG")
        psH1 = p.psum([128, 512], F32, "psH1")
        psH2 = p.psum([128, 512], F32, "psH2")
        for b in range(NB):
            tb = b * S
            for h in range(4):
                p.dma("sp", qT[:, h, :], c.qkT[h * 128:(h + 1) * 128, tb:tb + S], writes=[qT])
                p.dma("sp", kT[:, h, :], c.qkT[512 + h * 128:512 + (h + 1) * 128, tb:tb + S], writes=[kT])
                p.dma("sp", so[:, h, :], c.soT[h * 128:(h + 1) * 128, tb:tb + S], writes=[so])
            p.dma("sp", v[:], c.mv_tok[tb:tb + S, :].rearrange("(n q) f -> q n f", q=128), writes=[v])
            p.dma("sp", g[:], c.mg_tok[tb:tb + S, :].rearrange("(n q) f -> q n f", q=128), writes=[g])
            gb = cv(f"mgb{L}")
            tt(p, li[:], g[:, :, 0:4], gb[:, 0:4].unsqueeze(1).to_broadcast([128, NCH, 4]), ALU.add,
               [g, c.cvb], [li])
            tt(p, lf[:], g[:, :, 4:8], gb[:, 4:8].unsqueeze(1).to_broadcast([128, NCH, 4]), ALU.add,
               [g, c.cvb], [lf])
            act(p, lf[:], lf[:], AF.Exp, [lf], [lf], scale=-1.0)
            act(p, lf[:], lf[:], AF.Ln, [lf], [lf], bias=1.0)
            ts(p, lf[:], lf[:], -1.0, None, ALU.mult, None, [lf], [lf])
            for st_ in (CT, nb_):
                mset(p, st_[:], 0.0, [st_])
            for st_ in (CTb, nbb):
                mset(p, st_[:], 0.0, [st_])
            for ch in range(NCH):
                t0 = ch * 128
                sl = slice(t0, t0 + 128)
                pr = ch % 2
                R, WT, E, ST, qs, colb, a_ = R_[pr], WT_[pr], E_[pr], ST_[pr], qs_[pr], colb_[pr], a__[pr]
                dn, hr, sq, t1, ka = dn_[pr], hr_[pr], sq_[pr], t1_[pr], ka_[pr]
                tt(p, R[:].rearrange("q (h j) -> q h j", h=4), c.tri.unsqueeze(1).to_broadcast([128, 4, 128]),
                   lf[:, ch, :].unsqueeze(2).to_broadcast([128, 4, 128]), ALU.mult, [lf, c.cf], [R])
                mm(p, psA[:], c.ones_f, R[:], True, True, [R, c.cf], [psA])
                mm(p, psB[:, 0:4], c.tri, lf[:, ch, :], True, True, [lf, c.cf], [psB])
                tt(p, colb[:], li[:, ch, :], psB[:, 0:4], ALU.subtract, [li, psB], [colb])
                for h in range(4):
                    act(p, WT[:, h * 128:(h + 1) * 128], psA[:, h * 128:(h + 1) * 128], AF.Exp,
                        [psA, colb], [WT], bias=colb[:, h:h + 1])
                act(p, E[:], psA[:], AF.Exp, [psA], [E])
                for h in range(4):
                    act(p, a_[:, h:h + 1], psA[:, h * 128 + 127:h * 128 + 128], AF.Exp, [psA, colb], [a_],
                        bias=colb[:, h:h + 1])
                tt(p, WT[:], WT[:], c.mask4, ALU.mult, [WT, c.cf], [WT])
                for h in range(4):
                    mm(p, psC[:, h * 128:(h + 1) * 128], kT[:, h, sl], qT[:, h, sl], True, True,
                       [kT, qT], [psC])
                tt(p, ST[:], psC[:], WT[:], ALU.mult, [psC, WT], [ST])
                tt(p, qs[:], qT[:, :, sl], E[:].rearrange("q (h j) -> q h j", h=4), ALU.mult, [qT, E], [qs])
                for h in range(4):
                    hs = slice(h * 128, (h + 1) * 128)
                    mm(p, psD[:, hs], v[:, ch, hs], ST[:, hs], True, False, [v, ST], [psD])
                    mm(p, psD[:, hs], CTb[:, h, :], qs[:, h, :], False, True, [CTb, qs], [psD])
                    mm(p, psE[:, hs], c.ones_bf, ST[:, hs], True, False, [ST, c.cb], [psE])
                    mm(p, psE[:, hs], nbb[:, h, :], qs[:, h, :], False, True, [nbb, qs], [psE])
                act(p, dn[:], psE[:], AF.Abs, [psE], [dn])
                ts(p, dn[:], dn[:], 1.0, None, ALU.max, None, [dn], [dn])
                recip(p, dn[:], dn[:], [dn], [dn])
                tt(p, hr[:], psD[:], dn[:], ALU.mult, [psD, dn], [hr])
                act(p, sq[:], hr[:], AF.Square, [hr], [sq])
                mm(p, psB[:], c.ones_bf, sq[:], True, True, [sq, c.cb], [psB])
                act(p, t1[:], psB[:], AF.Sqrt, [psB, c.cvb], [t1], scale=1.0 / 128, bias=cv("eps"))
                recip(p, t1[:], t1[:], [t1], [t1])
                tt(p, hr[:], hr[:], t1[:], ALU.mult, [hr, t1], [hr])
                ng = cv(f"mng{L}")
                for h in range(4):
                    stt(p, hT[:, h, sl], hr[:, h * 128:(h + 1) * 128], ng[:, h:h + 1], so[:, h, sl],
                        ALU.mult, ALU.mult, [hr, so, c.cvb], [hT])
                for h in range(4):
                    tr(p, psG[:, h * 128:(h + 1) * 128], kT[:, h, sl], c.ident_bf, [kT, c.cb], [psG])
                for h in range(4):
                    ts(p, ka[:, h, :], psG[:, h * 128:(h + 1) * 128], a_[:, h:h + 1], None, ALU.mult, None,
                       [psG, a_], [ka])
                for h in range(4):
                    hs = slice(h * 128, (h + 1) * 128)
                    mm(p, psH1[:, hs], ka[:, h, :], v[:, ch, hs], True, True, [ka, v], [psH1])
                    mm(p, psH2[:, hs], ka[:, h, :], c.ones_bf, True, True, [ka, c.cb], [psH2])
                for h in range(4):
                    hs = slice(h * 128, (h + 1) * 128)
                    dcol = E[:, h * 128 + 127:h * 128 + 128]
                    stt(p, CT[:, h, :], CT[:, h, :], dcol, psH1[:, hs], ALU.mult, ALU.add, [CT, E, psH1], [CT])
                    stt(p, nb_[:, h, :], nb_[:, h, :], dcol, psH2[:, hs], ALU.mult, ALU.add, [nb_, E, psH2], [nb_])
                cp(p, CTb[:], CT[:], [CT], [CTb], eng="pool")
                cp(p, nbb[:], nb_[:], [nb_], [nbb], eng="pool")
            for h in range(4):
                p.dma("sp", c.mixT[h * 128:(h + 1) * 128, tb:tb + S], hT[:, h, :], reads=[hT])


def phase_ssd(p, c, L):
    S, NB = c.S, c.NB
    NCH = S // 128
    cv = c.cv
    with p.scope():
        xsT = p.sbuf([128, 8, S], BF16, "xsT")
        BT = p.sbuf([128, 2, S], BF16, "BT")
        CTm = p.sbuf([128, 2, S], BF16, "CTm")
        dtr = p.sbuf([128, NCH, 16], F32, "dtr")
        dt = p.sbuf([128, NCH, 16], F32, "dt")
        tmpd = p.sbuf([128, NCH, 16], F32, "tmpd")
        Aneg = p.sbuf([128, 16], F32, "Aneg")
        sng = p.sbuf([128, 1024], F32, "sng")
        yT = p.sbuf([128, 8, S], BF16, "yT")
        szb = [p.sbuf([128, 1024], F32, "sz") for _ in range(2)]
        hst = p.sbuf([128, 1024], F32, "hst")
        hstb = p.sbuf([128, 1024], BF16, "hstb")
        a_c_ = [p.sbuf([128, 16], F32, "a_c") for _ in range(2)]
        R2_ = [p.sbuf([128, 2048], F32, "R2") for _ in range(2)]
        acol_ = [p.sbuf([128, 16], F32, "acol") for _ in range(2)]
        tot_ = [p.sbuf([128, 16], F32, "tot") for _ in range(2)]
        Dm_ = [p.sbuf([128, 2048], F32, "Dm") for _ in range(2)]
        CBm_ = [p.sbuf([128, 256], F32, "CBm") for _ in range(2)]
        W_ = [p.sbuf([128, 2048], BF16, "W") for _ in range(2)]
        Xdt_ = [p.sbuf([128, 1024], BF16, "Xdt") for _ in range(2)]
        Xd_ = [p.sbuf([128, 1024], F32, "Xd") for _ in range(2)]
        Btok = p.sbuf([128, 256], BF16, "Btok")
        eac = p.sbuf([128, 16], F32, "eac")
        dte = p.sbuf([128, 16], F32, "dte")
        ecd = p.sbuf([128, 16], F32, "ecd")
        y = p.sbuf([128, 1024], F32, "y")
        ysq = p.sbuf([128, 1024], F32, "ysq")
        ss = p.sbuf([128, 2], F32, "ss")
        yn = p.sbuf([128, 1024], BF16, "yn")
        Xdd = p.sbuf([128, 1024], BF16, "Xdd")
        pb = [p.psum([128, 512], F32, f"pb{i}") for i in range(4)]
        pm = p.psum([128, 512], F32, "pm")
        px = p.psum([128, 1024], BF16, "px")
        py0 = p.psum([128, 512], F32, "py0")
        py1 = p.psum([128, 512], F32, "py1")
        p.dma("sp", sng[:], c.sng_d[L], writes=[sng])
        act(p, Aneg[:], cv(f"salog{L}"), AF.Exp, [c.cvb], [Aneg])
        ts(p, Aneg[:], Aneg[:], -1.0, None, ALU.mult, None, [Aneg], [Aneg])
        dsk = cv(f"sd{L}")
        for b in range(NB):
            tb = b * S
            for ch8 in range(8):
                p.dma("sp", xsT[:, ch8, :], c.xbcT[ch8 * 128:(ch8 + 1) * 128, tb:tb + S], writes=[xsT])
            for g_ in range(2):
                p.dma("sp", BT[:, g_, :], c.xbcT[1024 + g_ * 128:1024 + (g_ + 1) * 128, tb:tb + S], writes=[BT])
                p.dma("sp", CTm[:, g_, :], c.xbcT[1280 + g_ * 128:1280 + (g_ + 1) * 128, tb:tb + S], writes=[CTm])
            p.dma("sp", dtr[:], c.sdt_tok[tb:tb + S, :].rearrange("(n q) f -> q n f", q=128), writes=[dtr])
            tt(p, dtr[:], dtr[:], cv(f"sdtb{L}").unsqueeze(1).to_broadcast([128, NCH, 16]), ALU.add,
               [dtr, c.cvb], [dtr])
            act(p, tmpd[:], dtr[:], AF.Abs, [dtr], [tmpd])
            act(p, tmpd[:], tmpd[:], AF.Exp, [tmpd], [tmpd], scale=-1.0)
            act(p, tmpd[:], tmpd[:], AF.Ln, [tmpd], [tmpd], bias=1.0)
            stt(p, dt[:], dtr[:], 0.0, tmpd[:], ALU.max, ALU.add, [dtr, tmpd], [dt])
            mset(p, hst[:], 0.0, [hst])
            mset(p, hstb[:], 0.0, [hstb])
            for ch in range(NCH):
                t0 = ch * 128
                sl = slice(t0, t0 + 128)
                sz = szb[ch % 2]
                pr = ch % 2
                a_c, R2, acol, tot, Dm, CBm, W, Xdt, Xd = (a_c_[pr], R2_[pr], acol_[pr], tot_[pr], Dm_[pr],
                                                           CBm_[pr], W_[pr], Xdt_[pr], Xd_[pr])
                p.dma("sp", sz[:], c.sz_tok[tb + t0:tb + t0 + 128, :], writes=[sz])
                tt(p, a_c[:], dt[:, ch, :], Aneg[:], ALU.mult, [dt, Aneg], [a_c])
                for hq in range(2):
                    tt(p, R2[:, hq * 1024:(hq + 1) * 1024].rearrange("q (h j) -> q h j", h=8),
                       c.tri.unsqueeze(1).to_broadcast([128, 8, 128]),
                       a_c[:, hq * 8:(hq + 1) * 8].unsqueeze(2).to_broadcast([128, 8, 128]), ALU.mult,
                       [a_c, c.cf], [R2], eng=("dve" if hq else "pool"))
                for q in range(4):
                    mm(p, pb[q][:], c.ones_f, R2[:, q * 512:(q + 1) * 512], True, True, [R2, c.cf], [pb[q]])
                mm(p, pm[:, 0:16], c.tri, a_c[:], True, True, [a_c, c.cf], [pm])
                for g_ in range(2):
                    mm(p, pm[:, 128 + g_ * 128:256 + g_ * 128], BT[:, g_, sl], CTm[:, g_, sl], True, True,
                       [BT, CTm], [pm])
                cp(p, acol[:], pm[:, 0:16], [pm], [acol])
                tt(p, CBm[:], pm[:, 128:384], c.mask4[:, 0:256], ALU.mult, [pm, c.cf], [CBm])
                for q in range(4):
                    cp(p, tot[:, q * 4:(q + 1) * 4],
                       pb[q][:].rearrange("q (h j) -> q h j", h=4)[:, :, 127], [pb[q]], [tot])
                for h in range(16):
                    q, hh = h // 4, h % 4
                    ts(p, Dm[:, h * 128:(h + 1) * 128], pb[q][:, hh * 128:(hh + 1) * 128], acol[:, h:h + 1], 0.0,
                       ALU.subtract, ALU.min, [pb[q], acol], [Dm])
                act(p, Dm[:], Dm[:], AF.Exp, [Dm], [Dm])
                for g_ in range(2):
                    tt(p, W[:, g_ * 1024:(g_ + 1) * 1024].rearrange("q (h j) -> q h j", h=8),
                       Dm[:, g_ * 1024:(g_ + 1) * 1024].rearrange("q (h j) -> q h j", h=8),
                       CBm[:, g_ * 128:(g_ + 1) * 128].unsqueeze(1).to_broadcast([128, 8, 128]), ALU.mult,
                       [Dm, CBm], [W])
                for ch8 in range(8):
                    tr(p, px[:, ch8 * 128:(ch8 + 1) * 128], xsT[:, ch8, sl], c.ident_bf, [xsT, c.cb], [px])
                tt(p, Xdt[:].rearrange("q (h e) -> q h e", h=16), px[:].rearrange("q (h e) -> q h e", h=16),
                   dt[:, ch, :].unsqueeze(2).to_broadcast([128, 16, 64]), ALU.mult, [px, dt], [Xdt])
                tt(p, Xd[:].rearrange("q (h e) -> q h e", h=16), px[:].rearrange("q (h e) -> q h e", h=16),
                   dsk.unsqueeze(2).to_broadcast([128, 16, 64]), ALU.mult, [px, c.cvb], [Xd])
                for h in range(16):
                    py = py0 if h < 8 else py1
                    hh = h % 8
                    mm(p, py[:, hh * 64:(hh + 1) * 64], W[:, h * 128:(h + 1) * 128], Xdt[:, h * 64:(h + 1) * 64],
                       True, True, [W, Xdt], [py])
                for g_ in range(2):
                    mm(p, pb[g_][:], CTm[:, g_, sl], hstb[:, g_ * 512:(g_ + 1) * 512], True, True,
                       [CTm, hstb], [pb[g_]])
                act(p, eac[:], acol[:], AF.Exp, [acol], [eac])
                for g_ in range(2):
                    tt(p, y[:, g_ * 512:(g_ + 1) * 512].rearrange("q (h e) -> q h e", h=8),
                       pb[g_][:].rearrange("q (h e) -> q h e", h=8),
                       eac[:, g_ * 8:(g_ + 1) * 8].unsqueeze(2).to_broadcast([128, 8, 64]), ALU.mult,
                       [pb[g_], eac], [y])
                tt(p, y[:, 0:512], y[:, 0:512], py0[:], ALU.add, [y, py0], [y])
                tt(p, y[:, 512:1024], y[:, 512:1024], py1[:], ALU.add, [y, py1], [y])
                tt(p, y[:], y[:], Xd[:], ALU.add, [y, Xd], [y])
                tt(p, y[:], y[:], sz[:], ALU.mult, [y, sz], [y])
                for g_ in range(2):
                    act(p, ysq[:, g_ * 512:(g_ + 1) * 512], y[:, g_ * 512:(g_ + 1) * 512], AF.Square, [y], [ysq, ss],
                        accum_out=ss[:, g_:g_ + 1])
                act(p, ss[:], ss[:], AF.Sqrt, [ss, c.cvb], [ss], scale=1.0 / 512, bias=cv("eps"))
                recip(p, ss[:], ss[:], [ss], [ss])
                for g_ in range(2):
                    stt(p, yn[:, g_ * 512:(g_ + 1) * 512], y[:, g_ * 512:(g_ + 1) * 512], ss[:, g_:g_ + 1],
                        sng[:, g_ * 512:(g_ + 1) * 512], ALU.mult, ALU.mult, [y, ss, sng], [yn])
                for ch8 in range(8):
                    tr(p, px[:, ch8 * 128:(ch8 + 1) * 128], yn[:, ch8 * 128:(ch8 + 1) * 128], c.ident_bf,
                       [yn, c.cb], [px])
                cp(p, yT[:, :, sl], px[:].rearrange("q (c j) -> q c j", c=8), [px], [yT], eng="act")
                tt(p, dte[:], tot[:], acol[:], ALU.subtract, [tot, acol], [dte])
                act(p, dte[:], dte[:], AF.Exp, [dte], [dte])
                act(p, ecd[:], tot[:], AF.Exp, [tot], [ecd])
                tt(p, Xdd[:].rearrange("q (h e) -> q h e", h=16), Xdt[:].rearrange("q (h e) -> q h e", h=16),
                   dte[:].unsqueeze(2).to_broadcast([128, 16, 64]), ALU.mult, [Xdt, dte], [Xdd])
                for g_ in range(2):
                    tr(p, px[:, g_ * 128:(g_ + 1) * 128], BT[:, g_, sl], c.ident_bf, [BT, c.cb], [px])
                cp(p, Btok[:], px[:, 0:256], [px], [Btok])
                for g_ in range(2):
                    mm(p, pb[2 + g_][:], Btok[:, g_ * 128:(g_ + 1) * 128], Xdd[:, g_ * 512:(g_ + 1) * 512],
                       True, True, [Btok, Xdd], [pb[2 + g_]])
                tt(p, hst[:].rearrange("q (h e) -> q h e", h=16), hst[:].rearrange("q (h e) -> q h e", h=16),
                   ecd[:].unsqueeze(2).to_broadcast([128, 16, 64]), ALU.mult, [hst, ecd], [hst])
                for g_ in range(2):
                    tt(p, hst[:, g_ * 512:(g_ + 1) * 512], hst[:, g_ * 512:(g_ + 1) * 512], pb[2 + g_][:], ALU.add,
                       [hst, pb[2 + g_]], [hst])
                cp(p, hstb[:], hst[:], [hst], [hstb], eng="pool")
            for ch8 in range(8):
                p.dma("sp", c.mixT[512 + ch8 * 128:512 + (ch8 + 1) * 128, tb:tb + S], yT[:, ch8, :], reads=[yT])


def phase_moba(p, c, L):
    S, NB = c.S, c.NB
    NQT = S // 128
    NBLK = S // 256
    with p.scope():
        qT = p.sbuf([128, 4, S], BF16, "aq")
        kT = p.sbuf([128, 4, S], BF16, "ak")
        v = p.sbuf([128, NQT, 512], BF16, "av")
        oT = p.sbuf([128, 4, S], BF16, "oT")
        km = p.sbuf([128, 8], F32, "km")
        kmb = p.sbuf([128, 8], BF16, "kmb")
        sc = p.sbuf([128, 8], F32, "sc")
        top8 = p.sbuf([128, 8], F32, "top8")
        brow = p.sbuf([128, 8], F32, "brow")
        biasT = p.sbuf([8, S], BF16, "biasT")
        scA = p.sbuf([128, 128], F32, "scA")
        topA = p.sbuf([128, 128], F32, "topA")
        browA = p.sbuf([128, 128], F32, "browA")
        PT = [p.sbuf([128, 512], BF16, "PT") for _ in range(2)]
        rden = p.sbuf([128, 512], F32, "rden")
        pS = [p.psum([128, 512], F32, f"pS{i}") for i in range(2)]
        pO = p.psum([128, 512], F32, "pO")
        pD = p.psum([128, 512], F32, "pD")
        pq = p.psum([128, 512], F32, "pq")
        pt = p.psum([128, 512], F32, "ptr")
        sel8 = c.cbv("sel8", 8)
        mr = c.cbv("mr")
        k = 0
        for b in range(NB):
            tb = b * S
            for h in range(4):
                p.dma("sp", qT[:, h, :], c.aqT[h * 128:(h + 1) * 128, tb:tb + S], writes=[qT])
                p.dma("sp", kT[:, h, :], c.akT[h * 128:(h + 1) * 128, tb:tb + S], writes=[kT])
            p.dma("sp", v[:], c.av_tok[tb:tb + S, :].rearrange("(n q) f -> q n f", q=128), writes=[v])
            for h in range(4):
                mset(p, km[:], 0.0, [km])
                p.op("dve", lambda e, h=h: e.tensor_reduce(out=km[:, 0:NBLK],
                                                          in_=kT[:, h, :].rearrange("q (n j) -> q n j", j=256),
                                                          axis=AX.X, op=ALU.add), [kT], [km])
                ts(p, kmb[:], km[:], 1.0 / 256, None, ALU.mult, None, [km], [kmb])
                for qt in range(NQT):
                    mm(p, pq[:, qt * 8:(qt + 1) * 8], qT[:, h, qt * 128:(qt + 1) * 128], kmb[:], True, True,
                       [qT, kmb], [pq])
                NC8 = NQT * 8
                tt(p, scA[:, 0:NC8], pq[:, 0:NC8], c.cfv("vmask")[:, 0:NC8], ALU.add, [pq, c.cf], [scA])
                for qt in range(NQT):
                    p.op("dve", lambda e, qt=qt: e.max(out=topA[:, qt * 8:(qt + 1) * 8], in_=scA[:, qt * 8:(qt + 1) * 8]),
                         [scA], [topA])
                tt(p, browA[:, 0:NC8].rearrange("q (t n) -> q t n", n=8),
                   scA[:, 0:NC8].rearrange("q (t n) -> q t n", n=8),
                   topA[:, 0:NC8].rearrange("q (t n) -> q t n", n=8)[:, :, 2:3].to_broadcast([128, NQT, 8]),
                   ALU.is_ge, [scA, topA], [browA])
                ts(p, browA[:, 0:NC8], browA[:, 0:NC8], -NEG, NEG, ALU.mult, ALU.add, [browA], [browA])
                tt(p, browA[:, 0:NC8], browA[:, 0:NC8], c.cfv("ownmask")[:, 0:NC8], ALU.mult, [browA, c.cf], [browA])
                for q4 in range(NQT // 4):
                    for j in range(4):
                        qt = q4 * 4 + j
                        tr(p, pt[0:8, j * 128:(j + 1) * 128], browA[:, qt * 8:(qt + 1) * 8], c.ident_f,
                           [browA, c.cf], [pt])
                    cp(p, biasT[:, q4 * 512:(q4 + 1) * 512], pt[0:8, :], [pt], [biasT], eng="act")
                for jt in range(S // 512):
                    js = slice(jt * 512, (jt + 1) * 512)
                    nst = 4 * jt + 4
                    def emit_S(st, kk):
                        ps = pS[kk % 2]
                        diag = st >= 4 * jt
                        mm(p, ps[:], kT[:, h, st * 128:(st + 1) * 128], qT[:, h, js], True, False, [kT, qT], [ps])
                        mm(p, ps[:], sel8[:, (st // 2) * 128:(st // 2 + 1) * 128], biasT[:, js], False, not diag,
                           [biasT, c.cb], [ps])
                        if diag:
                            r = st - 4 * jt
                            mm(p, ps[:], c.ident_bf, mr[:, r * 512:(r + 1) * 512], False, True, [c.cb], [ps])
                    emit_S(0, k)
                    for st in range(nst):
                        ps = pS[k % 2]
                        pt_ = PT[k % 2]
                        if st + 1 < nst:
                            emit_S(st + 1, k + 1)
                        act(p, pt_[:], ps[:], AF.Exp, [ps], [pt_])
                        mm(p, pO[:], v[:, st, h * 128:(h + 1) * 128], pt_[:], st == 0, st == nst - 1, [v, pt_], [pO])
                        mm(p, pD[:], c.ones_bf, pt_[:], st == 0, st == nst - 1, [pt_, c.cb], [pD])
                        k += 1
                    recip(p, rden[:], pD[:], [pD], [rden])
                    tt(p, oT[:, h, js], pO[:], rden[:], ALU.mult, [pO, rden], [oT])
            for h in range(4):
                p.dma("sp", c.mixT[1536 + h * 128:1536 + (h + 1) * 128, tb:tb + S], oT[:, h, :], reads=[oT])


def load_w_cast(p, wbuf_ap, src_ap, wbuf, nsplit=4, axis_len=None):
    n = axis_len
    step = max(1, n // nsplit)
    for q0 in range(0, n, step):
        q1 = min(n, q0 + step)
        p.dma("pool", wbuf_ap[:, q0:q1, :], src_ap[:, q0:q1, :], writes=[wbuf])


def store_xt(p, dst_d, xt, t0, n=512):
    v = dst_d.rearrange("(kc q) t -> q kc t", q=128)
    for q in range(4):
        p.dma("sp", v[:, q * 4:(q + 1) * 4, t0:t0 + n], xt[:, q * 4:(q + 1) * 4, :], reads=[xt])


def phase_outproj(p, c, L, x_in, x_out):
    T = c.T
    with p.scope():
        wo = p.sbuf([128, KC, D], BF16, "wo")
        load_w_cast(p, wo, c.w_out[L].rearrange("(kc q) n -> q kc n", q=128), wo, 8, KC)
        mt = [p.sbuf([128, KC, 512], BF16, "mt") for _ in range(2)]
        xt = [p.sbuf([128, KC, 512], F32, "xt") for _ in range(2)]
        pss = [p.psum([128, 512], F32, f"po{i}") for i in range(4)]
        mv = c.mixT.rearrange("(kc q) t -> q kc t", q=128)
        def issue(ti):
            t0 = ti * 512
            for q in range(2):
                p.dma("sp", mt[ti % 2][:, q * 8:(q + 1) * 8, :], mv[:, q * 8:(q + 1) * 8, t0:t0 + 512],
                      writes=[mt[ti % 2]])
            load_xt(p, xt[ti % 2], x_in, t0)
        issue(0)
        for ti in range(T // 512):
            t0 = ti * 512
            m = mt[ti % 2]
            x = xt[ti % 2]
            if ti + 1 < T // 512:
                issue(ti + 1)
            for dc in range(KC):
                ps = pss[dc % 4]
                for kc in range(KC):
                    mm(p, ps[:], wo[:, kc, dc * 128:(dc + 1) * 128], m[:, kc, :], kc == 0, kc == KC - 1, [wo, m], [ps])
                tt(p, x[:, dc, :], x[:, dc, :], ps[:], ALU.add, [x, ps], [x])
            store_xt(p, x_out, x, t0)


def phase_ffn(p, c, x_in, x_out, moe):
    T = c.T
    L = 1 if moe else 0
    NF = 56 if moe else 44
    NE = 8 if moe else 1
    NS = 2 if T % 1024 == 0 else 1
    NG = 4
    GF = NF // NG
    TT = NS * 512
    with p.scope():
        xt = [p.sbuf([128, KC, 512], F32, "xt") for _ in range(NS)]
        xn = [p.sbuf([128, KC, 512], BF16, "xn") for _ in range(NS)]
        hraw = p.sbuf([128, 8192], F32, "hraw")
        hT = hraw.t[:, 0:NS * GF * 256].bitcast(BF16).rearrange("q (s f t) -> q s f t", s=NS, t=512)
        sq = hraw.t[:, 0:4096].bitcast(BF16).rearrange("q (f t) -> q f t", t=512)
        xn32 = hraw.t[:, 0:8192].rearrange("q (f t) -> q f t", t=512)
        tmp = p.sbuf([128, 512], F32, "tmp")
        rstd = p.sbuf([128, 512], F32, "rstd")
        gsb = p.sbuf([128, 512], F32, "gsb")
        BW = 2 if GF % 2 == 0 else 1
        wg = [p.sbuf([128, KC, 128 * BW], BF16, "wg") for _ in range(2)]
        wu = [p.sbuf([128, KC, 128 * BW], BF16, "wu") for _ in range(2)]
        wd = [p.sbuf([128, GF, 256], BF16, "wd") for _ in range(2)]
        pg = [p.psum([128, 512], F32, f"pg{i}") for i in range(2)]
        pu = [p.psum([128, 512], F32, f"pu{i}") for i in range(2)]
        pd = [p.psum([128, 512], F32, f"pd{i}") for i in range(2)]
        pn = p.psum([128, 512], F32, "pn")
        px = p.psum([128, 512], F32, "pxx")
        if moe:
            wr = p.sbuf([128, KC, 8], F32, "wr")
            p.dma("sp", wr[:], c.moe_r[0].rearrange("(kc q) n -> q kc n", q=128), writes=[wr])
            lg = p.sbuf([128, 4, 8], F32, "lg")
            top8 = p.sbuf([128, 8], F32, "top8")
            cmb = p.sbuf([128, 4, 8], F32, "cmb")
            ssum = p.sbuf([128, 1], F32, "ssum")
            nmx = p.sbuf([128, 1], F32, "nmx")
            combT = [p.sbuf([8, 512], F32, "combT") for _ in range(NS)]
            cbe = [p.sbuf([128, 512], F32, "cbe") for _ in range(NS)]
            sel8f = c.cfv("sel8", 8)
        gcols = c.cv(f"g_ffn{L}")
        wi = 0
        di = 0
        pi = 0
        for ti in range(T // TT):
            for s_ in range(NS):
                t0 = ti * TT + s_ * 512
                x = xt[s_]
                load_xt(p, x, x_in, t0)
                act(p, sq, x[:], AF.Square, [x], [hraw])
                for kc in range(KC):
                    mm(p, pn[:], c.ones_bf, sq[:, kc, :], kc == 0, kc == KC - 1, [hraw, c.cb], [pn])
                act(p, tmp[:], pn[:], AF.Sqrt, [pn, c.cvb], [tmp], scale=1.0 / D, bias=c.cv("eps"))
                recip(p, rstd[:], tmp[:], [tmp], [rstd])
                for kc in range(KC):
                    stt(p, xn[s_][:, kc, :], x[:, kc, :], gcols[:, kc:kc + 1], rstd[:], ALU.mult, ALU.mult,
                        [x, rstd, c.cvb], [xn[s_]])
                if moe:
                    for kc in range(KC):
                        stt(p, xn32[:, kc, :], x[:, kc, :], gcols[:, kc:kc + 1], rstd[:], ALU.mult, ALU.mult,
                            [x, rstd, c.cvb], [hraw])
                    for sub in range(4):
                        for kc in range(KC):
                            mm(p, px[:, sub * 8:(sub + 1) * 8], xn32[:, kc, sub * 128:(sub + 1) * 128], wr[:, kc, :],
                               kc == 0, kc == KC - 1, [hraw, wr], [px])
                    cp(p, lg[:], px[:, 0:32].rearrange("q (s e) -> q s e", e=8), [px], [lg])
                    for sub in range(4):
                        p.op("dve", lambda e, sub=sub: e.max(out=top8[:], in_=lg[:, sub, :]), [lg], [top8])
                        ts(p, nmx[:], top8[:, 0:1], -1.0, None, ALU.mult, None, [top8], [nmx])
                        act(p, cmb[:, sub, :], lg[:, sub, :], AF.Exp, [lg, nmx], [cmb], bias=nmx[:, 0:1])
                        stt(p, cmb[:, sub, :], lg[:, sub, :], top8[:, 1:2], cmb[:, sub, :], ALU.is_ge, ALU.mult,
                            [lg, top8, cmb], [cmb])
                        p.op("dve", lambda e, sub=sub: e.tensor_reduce(out=ssum[:], in_=cmb[:, sub, :], axis=AX.X,
                                                                      op=ALU.add), [cmb], [ssum])
                        recip(p, ssum[:], ssum[:], [ssum], [ssum])
                        ts(p, cmb[:, sub, :], cmb[:, sub, :], ssum[:, 0:1], None, ALU.mult, None, [cmb, ssum], [cmb])
                        tr(p, pn[0:8, sub * 128:(sub + 1) * 128], cmb[:, sub, :], c.ident_f, [cmb, c.cf], [pn])
                    cp(p, combT[s_][:], pn[0:8, :], [pn], [combT[s_]])
            for e_ in range(NE):
                if moe:
                    wgv = c.moe_wg[0, e_].rearrange("(kc q) n -> q kc n", q=128)
                    wuv = c.moe_wu[0, e_].rearrange("(kc q) n -> q kc n", q=128)
                    wdv = c.moe_wd[0, e_].rearrange("(f q) n -> q f n", q=128)
                    for s_ in range(NS):
                        mm(p, pn[:], sel8f[:, e_ * 128:(e_ + 1) * 128], combT[s_][:], True, True, [combT[s_], c.cf], [pn])
                        cp(p, cbe[s_][:], pn[:], [pn], [cbe[s_]], eng="act")
                else:
                    wgv = c.ffn_wg[0].rearrange("(kc q) n -> q kc n", q=128)
                    wuv = c.ffn_wu[0].rearrange("(kc q) n -> q kc n", q=128)
                    wdv = c.ffn_wd[0].rearrange("(f q) n -> q f n", q=128)
                for gi in range(NG):
                    for fp in range(GF // BW):
                        fc0 = gi * GF + fp * BW
                        g_, u_ = wg[wi % 2], wu[wi % 2]
                        wi += 1
                        load_w_cast(p, g_, wgv[:, :, fc0 * 128:(fc0 + BW) * 128], g_, 2, KC)
                        load_w_cast(p, u_, wuv[:, :, fc0 * 128:(fc0 + BW) * 128], u_, 2, KC)
                        for s_ in range(NS):
                            for j in range(BW):
                                fl = fp * BW + j
                                js = slice(j * 128, (j + 1) * 128)
                                a, b2 = pg[pi % 2], pu[pi % 2]
                                pi += 1
                                for kc in range(KC):
                                    mm(p, a[:], g_[:, kc, js], xn[s_][:, kc, :], kc == 0, kc == KC - 1,
                                       [g_, xn[s_]], [a])
                                for kc in range(KC):
                                    mm(p, b2[:], u_[:, kc, js], xn[s_][:, kc, :], kc == 0, kc == KC - 1,
                                       [u_, xn[s_]], [b2])
                                act(p, gsb[:], a[:], AF.Silu, [a], [gsb])
                                if moe:
                                    tt(p, gsb[:], gsb[:], cbe[s_][:], ALU.mult, [gsb, cbe[s_]], [gsb])
                                tt(p, hT[:, s_, fl, :], gsb[:], b2[:], ALU.mult, [gsb, b2], [hraw])
                    for dp in range(KC // 2):
                        d_ = wd[di % 2]
                        di += 1
                        load_w_cast(p, d_, wdv[:, gi * GF:(gi + 1) * GF, dp * 256:(dp + 1) * 256], d_, 2, GF)
                        for s_ in range(NS):
                            for j in range(2):
                                dc = dp * 2 + j
                                o = pd[pi % 2]
                                pi += 1
                                for fl in range(GF):
                                    mm(p, o[:], d_[:, fl, j * 128:(j + 1) * 128], hT[:, s_, fl, :], fl == 0,
                                       fl == GF - 1, [d_, hraw], [o])
                                tt(p, xt[s_][:, dc, :], xt[s_][:, dc, :], o[:], ALU.add, [xt[s_], o], [xt[s_]])
            for s_ in range(NS):
                store_xt(p, x_out, xt[s_], ti * TT + s_ * 512)


def phase_ple(p, c, L, x_in, x_out, final):
    T = c.T
    with p.scope():
        wgt = p.sbuf([128, KC, D], BF16, "wgt")
        wp = p.sbuf([128, 2, D], BF16, "wp")
        load_w_cast(p, wgt, c.ple_gate[L].rearrange("(kc q) n -> q kc n", q=128), wgt, 8, KC)
        load_w_cast(p, wp, c.ple_proj[L].rearrange("(kc q) n -> q kc n", q=128), wp, 1, 2)
        xt = [p.sbuf([128, KC, 512], F32, "xt") for _ in range(2)]
        xn = p.sbuf([128, KC, 512], BF16, "xn")
        sq = p.sbuf([128, KC, 512], BF16, "sq")
        pt = [p.sbuf([128, 2, 512], BF16, "pt") for _ in range(2)]
        tmp = p.sbuf([128, 512], F32, "tmp")
        rstd = p.sbuf([128, 512], F32, "rstd")
        sg = p.sbuf([128, 512], F32, "sg")
        pn = p.psum([128, 512], F32, "pn")
        pgs = [p.psum([128, 512], F32, f"pg{i}") for i in range(2)]
        pps = [p.psum([128, 512], F32, f"pp{i}") for i in range(2)]
        gcols = c.cv(f"g_ple{L}")
        pv = c.pT[L].rearrange("(kc q) t -> q kc t", q=128)
        def issue(ti):
            load_xt(p, xt[ti % 2], x_in, ti * 512)
            p.dma("pool", pt[ti % 2][:], pv[:, :, ti * 512:ti * 512 + 512], writes=[pt[ti % 2]])
        issue(0)
        for ti in range(T // 512):
            t0 = ti * 512
            x = xt[ti % 2]
            pp_ = pt[ti % 2]
            if ti + 1 < T // 512:
                issue(ti + 1)
            norm_tile(p, c, x, gcols, lambda kc: xn[:, kc, :], sq, pn, tmp, rstd, [xn])
            for dc in range(KC):
                a, b2 = pgs[dc % 2], pps[dc % 2]
                for kc in range(KC):
                    mm(p, a[:], wgt[:, kc, dc * 128:(dc + 1) * 128], xn[:, kc, :], kc == 0, kc == KC - 1, [wgt, xn], [a])
                for kc in range(2):
                    mm(p, b2[:], wp[:, kc, dc * 128:(dc + 1) * 128], pp_[:, kc, :], kc == 0, kc == 1, [wp, pp_], [b2])
                act(p, sg[:], a[:], AF.Sigmoid, [a], [sg])
                tt(p, sg[:], sg[:], b2[:], ALU.mult, [sg, b2], [sg])
                tt(p, x[:, dc, :], x[:, dc, :], sg[:], ALU.add, [x, sg], [x], eng="pool")
            if final:
                act(p, sq[:], x[:], AF.Square, [x], [sq])
                for kc in range(KC):
                    mm(p, pn[:], c.ones_bf, sq[:, kc, :], kc == 0, kc == KC - 1, [sq, c.cb], [pn])
                act(p, tmp[:], pn[:], AF.Sqrt, [pn, c.cvb], [tmp], scale=1.0 / D, bias=c.cv("eps"))
                recip(p, rstd[:], tmp[:], [tmp], [rstd])
                gf = c.cv("g_fin")
                for kc in range(KC):
                    stt(p, x[:, kc, :], x[:, kc, :], gf[:, kc:kc + 1], rstd[:], ALU.mult, ALU.mult,
                        [x, rstd, c.cvb], [x])
            store_xt(p, x_out, x, t0)


class Layout:
    def __init__(self):
        self.off = {}
        self.w = 0

    def add(self, name, width):
        self.off[name] = (self.w, width)
        self.w += width


def cv_layout():
    l = Layout()
    for L in range(2):
        for nm, w in (("g_mix", 16), ("g_ffn", 16), ("g_ple", 16), ("mcw", 32), ("mcb", 8), ("scw", 48),
                      ("scb", 12), ("mng", 4), ("mgb", 8), ("sdtb", 16), ("salog", 16), ("sd", 16)):
            l.add(f"{nm}{L}", w)
    l.add("g_fin", 16)
    l.add("eps", 1)
    return l


def cf_layout():
    l = Layout()
    for nm, w in (("ones", 128), ("ident", 128), ("tri", 128), ("mask4", 512), ("sel8", 1024),
                  ("vmask", 128), ("ownmask", 128)):
        l.add(nm, w)
    return l


def cb_layout():
    l = Layout()
    for nm, w in (("ones", 128), ("ident", 128), ("mr", 2048), ("sel8", 1024)):
        l.add(nm, w)
    return l


CVL, CFL, CBL = cv_layout(), cf_layout(), cb_layout()


def cols(v, n):
    return np.ascontiguousarray(np.asarray(v, np.float32).reshape(n, 128).T)


def pack_consts(inp):
    cv = np.zeros((128, CVL.w), np.float32)

    def put(name, arr):
        o, w = CVL.off[name]
        cv[:, o:o + w] = arr
    for L in range(2):
        put(f"g_mix{L}", cols(inp["ln_mix"][L], 16))
        put(f"g_ffn{L}", cols(inp["ln_ffn"][L], 16))
        put(f"g_ple{L}", cols(inp["ln_ple"][L], 16))
        w = np.asarray(inp["m_conv_w"][L], np.float32)
        put(f"mcw{L}", w.T.reshape(8, 128, 4).transpose(1, 0, 2).reshape(128, 32))
        put(f"mcb{L}", cols(inp["m_conv_b"][L], 8))
        w = np.asarray(inp["s_conv_w"][L], np.float32)
        put(f"scw{L}", w.T.reshape(12, 128, 4).transpose(1, 0, 2).reshape(128, 48))
        put(f"scb{L}", cols(inp["s_conv_b"][L], 12))
        put(f"mng{L}", cols(inp["m_norm_g"][L], 4))
        put(f"mgb{L}", np.broadcast_to(np.asarray(inp["m_gate_b"][L], np.float32)[None, :], (128, 8)))
        put(f"sdtb{L}", np.broadcast_to(np.asarray(inp["s_dt_bias"][L], np.float32)[None, :], (128, 16)))
        put(f"salog{L}", np.broadcast_to(np.asarray(inp["s_a_log"][L], np.float32)[None, :], (128, 16)))
        put(f"sd{L}", np.broadcast_to(np.asarray(inp["s_d"][L], np.float32)[None, :], (128, 16)))
    put("g_fin", cols(inp["ln_final"], 16))
    put("eps", np.full((128, 1), EPS, np.float32))

    cf = np.zeros((128, CFL.w), np.float32)
    tri = (np.arange(128)[:, None] <= np.arange(128)[None, :]).astype(np.float32)
    sel8 = np.zeros((128, 8, 128), np.float32)
    for k in range(8):
        sel8[k, k, :] = 1.0
    qbv = np.arange(16)[:, None] // 2
    nv = np.arange(8)[None, :]
    vmask = np.where(nv < qbv, 0.0, -1e30).astype(np.float32).reshape(1, 128)
    ownmask = np.where(nv == qbv, 0.0, 1.0).astype(np.float32).reshape(1, 128)
    for nm, arr in (("ones", np.ones((128, 128))), ("ident", np.eye(128)), ("tri", tri),
                    ("mask4", np.tile(tri, (1, 4))), ("sel8", sel8.reshape(128, 1024)),
                    ("vmask", np.broadcast_to(vmask, (128, 128))), ("ownmask", np.broadcast_to(ownmask, (128, 128)))):
        o, w = CFL.off[nm]
        cf[:, o:o + w] = arr
    cbf = np.zeros((128, CBL.w), np.float32)
    mr = np.zeros((128, 4, 512), np.float32)
    for r in range(4):
        mr[:, r, :] = np.where(r * 128 + np.arange(128)[:, None] > np.arange(512)[None, :], NEG, 0.0)
    for nm, arr in (("ones", np.ones((128, 128))), ("ident", np.eye(128)), ("mr", mr.reshape(128, 2048)),
                    ("sel8", sel8.reshape(128, 1024))):
        o, w = CBL.off[nm]
        cbf[:, o:o + w] = arr
    return cv, cf, cbf.astype(ml_dtypes.bfloat16)


def build(S=2048, NB=2, stages=("all",), depth=2, dbg=()):
    T = NB * S
    nc = bass.Bass("TRN2", target_bir_lowering=False)
    c = Ctx()
    c.S, c.NB, c.T = S, NB, T

    c.declared = []

    def din(name, shape, dt=F32, need=True):
        if not need:
            return None
        c.declared.append(name)
        return nc.dram_tensor(name, list(shape), dt, kind="ExternalInput").ap()
    stages = set(stages)
    allst = "all" in stages
    nffn = allst or "ffn" in stages
    nmoe = (allst or "moe" in stages) and depth > 1
    nple = allst or "ple" in stages

    def dscr(name, shape, dt):
        if name in dbg:
            return nc.dram_tensor(name, list(shape), dt, kind="ExternalOutput").ap()
        return nc.dram_tensor(name, list(shape), dt).ap()

    xT = din("xT", [D, T])
    c.pT = din("pT", [2, 256, T])
    c.w_in = din("w_in", [2, D, DPROJ])
    c.w_out = din("w_out", [2, D, D])
    c.ffn_wg = din("ffn_w_gate", [1, D, 5632], need=nffn)
    c.ffn_wu = din("ffn_w_up", [1, D, 5632], need=nffn)
    c.ffn_wd = din("ffn_w_down", [1, 5632, D], need=nffn)
    c.moe_r = din("moe_router", [1, D, 8], need=nmoe)
    c.moe_wg = din("moe_w_gate", [1, 8, D, 7168], need=nmoe)
    c.moe_wu = din("moe_w_up", [1, 8, D, 7168], need=nmoe)
    c.moe_wd = din("moe_w_down", [1, 8, 7168, D], need=nmoe)
    c.ple_proj = din("ple_proj", [2, 256, D], need=nple)
    c.ple_gate = din("ple_gate", [2, D, D], need=nple)
    c.sng_d = din("sng", [2, 128, 1024])
    cvec = din("cvec", [128, CVL.w])
    cf32 = din("cf32", [128, CFL.w])
    cbf = din("cbf", [128, CBL.w], BF16)
    yT = nc.dram_tensor("yT", [D, T], F32, kind="ExternalOutput").ap()

    c.qk_pre = dscr("qk_pre", [1024, T], F32)
    c.qkT = dscr("qkT", [1024, T], BF16)
    c.mv_tok = dscr("mv_tok", [T, 512], BF16)
    c.soT = dscr("soT", [512, T], F32)
    c.mg_tok = dscr("mg_tok", [T, 8], F32)
    c.sz_tok = dscr("sz_tok", [T, 1024], F32)
    c.xbc_pre = dscr("xbc_pre", [1536, T], F32)
    c.xbcT = dscr("xbcT", [1536, T], BF16)
    c.sdt_tok = dscr("sdt_tok", [T, 16], F32)
    c.aqT = dscr("aqT", [512, T], BF16)
    c.akT = dscr("akT", [512, T], BF16)
    c.av_tok = dscr("av_tok", [T, 512], BF16)
    c.mixT = dscr("mixT", [D, T], BF16)
    c.xs = [dscr("xA", [D, T], F32), dscr("xB", [D, T], F32), dscr("xC", [D, T], F32)]

    p = Prog(nc)
    c.cvb = p.sbuf([128, CVL.w], F32, "cvec")
    c.cf = p.sbuf([128, CFL.w], F32, "cf32")
    c.cb = p.sbuf([128, CBL.w], BF16, "cbf")
    p.dma("sp", c.cvb[:], cvec, writes=[c.cvb])
    p.dma("sp", c.cf[:], cf32, writes=[c.cf])
    p.dma("sp", c.cb[:], cbf, writes=[c.cb])

    def cv(name):
        o, w = CVL.off[name]
        return c.cvb.t[:, o:o + w]

    def cfv(name, np_=128):
        o, w = CFL.off[name]
        return c.cf.t[0:np_, o:o + w]

    def cbv(name, np_=128):
        o, w = CBL.off[name]
        return c.cb.t[0:np_, o:o + w]
    c.cv, c.cfv, c.cbv = cv, cfv, cbv
    c.ones_bf = cbv("ones")
    c.ident_bf = cbv("ident")
    c.ones_f = cfv("ones")
    c.ident_f = cfv("ident")
    c.tri = cfv("tri")
    c.mask4 = cfv("mask4")

    x_cur = xT
    for L in range(depth):
        if allst or "inproj" in stages:
            phase_inproj(p, c, L, x_cur)
        if allst or "conv" in stages:
            phase_conv(p, c, L)
        if allst or "mlstm" in stages:
            phase_mlstm(p, c, L)
        if allst or "ssd" in stages:
            phase_ssd(p, c, L)
        if allst or "moba" in stages:
            phase_moba(p, c, L)
        if allst or "outproj" in stages:
            phase_outproj(p, c, L, x_cur, c.xs[0])
            x_cur = c.xs[0]
        if allst or "ffn" in stages:
            phase_ffn(p, c, x_cur, c.xs[1], moe=(L == 1))
            x_cur = c.xs[1]
        if allst or "ple" in stages:
            last = (L == depth - 1)
            phase_ple(p, c, L, x_cur, yT if last else c.xs[2], final=last)
            x_cur = c.xs[2]
    p.finish()
    nc._declared_inputs = list(c.declared)
    return nc


def host_inputs(inputs, S, NB, ncores, nc=None):
    cv, cf, cbf = pack_consts(inputs)
    x = np.asarray(inputs["x"], np.float32)
    pp = np.asarray(inputs["p"], np.float32)
    shared = {"cvec": cv, "cf32": cf, "cbf": cbf,
              "sng": np.ascontiguousarray(np.broadcast_to(np.asarray(inputs["s_norm_g"], np.float32)[:, None, :], (2, 128, 1024)))}
    for k in ("w_in", "w_out", "ffn_w_gate", "ffn_w_up", "ffn_w_down", "moe_router", "moe_w_gate",
              "moe_w_up", "moe_w_down", "ple_proj", "ple_gate"):
        shared[k] = np.ascontiguousarray(np.asarray(inputs[k], np.float32))
    maps = []
    for i in range(ncores):
        xb = x[i * NB:(i + 1) * NB].reshape(NB * S, D)
        m = dict(shared)
        m["xT"] = np.ascontiguousarray(xb.T)
        m["pT"] = np.ascontiguousarray(pp[:, i * NB:(i + 1) * NB].reshape(2, NB * S, 256).transpose(0, 2, 1))
        if nc is not None:
            m = {k: v for k, v in m.items() if k in nc._declared_inputs}
        maps.append(m)
    return maps


def kernel(**inputs):
    S, NB, ncores = 2048, 2, 8
    nc = build(S, NB)
    maps = host_inputs(inputs, S, NB, ncores, nc)
    res = run_bass_kernel_spmd(nc, maps, core_ids=list(range(ncores)))
    outs = [np.asarray(r["yT"]).T.reshape(NB, S, D) for r in res.results]
    return np.concatenate(outs, axis=0).astype(np.float32)
```
